# Optimizing a Trainium2 kernel written in Bass

```python
import math
import jax, jax.numpy as jnp
from jax import lax
import numpy as np

D_MODEL = 1024
BATCH = 4
SEQ = 4096
DEPTH = 4

N_MIXERS = 3
A_GROUPS = ((128, 1), (512, 4), (2048, 16))
A_HEADS = 16
A_HEAD_DIM = D_MODEL // A_HEADS
A_BLOCK = 128
NUM_BUCKETS = 32
MAX_DISTANCE = 2048
B_CHUNK = 128
B_WIDTH = 2 * D_MODEL
B_GROUPS = 16
C_HEADS = 8
C_HEAD_DIM = D_MODEL // C_HEADS
C_CONV = 4
C_CHUNK = 64
MOE_GROUPS = 8
MOE_PER_GROUP = 8
MOE_EXPERTS = MOE_GROUPS * MOE_PER_GROUP
MOE_TOPK = 2
MOE_HIDDEN = D_MODEL // 2
LN_EPS = 1e-5
RMS_EPS = 1e-6
DEEPNORM_ALPHA = (2 * DEPTH) ** 0.25
DEEPNORM_BETA = (8 * DEPTH) ** -0.25
N_A = (DEPTH + 2) // 3
N_B = (DEPTH + 1) // 3
N_C = DEPTH // 3

kernel_name = 'hybrid_dilated_sgu_deltanet_hmoe'


def layer_norm(x, g, b):
    xf = x.astype(jnp.float32)
    mu = jnp.mean(xf, axis=-1, keepdims=True)
    xc = xf - mu
    var = jnp.mean(xc * xc, axis=-1, keepdims=True)
    y = xc * lax.rsqrt(var + LN_EPS) * g.astype(jnp.float32) + b.astype(jnp.float32)
    return y.astype(x.dtype)


def t5_bucket(dist):
    max_exact = NUM_BUCKETS // 2
    d = jnp.maximum(dist, 1).astype(jnp.float32)
    large = max_exact + (jnp.log(d / max_exact) / math.log(MAX_DISTANCE / max_exact)
                         * (NUM_BUCKETS - max_exact)).astype(jnp.int32)
    return jnp.where(dist < max_exact, dist, jnp.minimum(large, NUM_BUCKETS - 1))


def _dilated_group(q, k, v, window, dil, rel_bias):
    B, S, H, Dh = q.shape
    steps = window // dil
    L = S // dil
    nb = -(-L // A_BLOCK)
    Lp = nb * A_BLOCK

    def by_residue(t):
        t = t.reshape(B, L, dil, H, Dh).transpose(0, 2, 3, 1, 4)
        return jnp.pad(t, ((0, 0), (0, 0), (0, 0), (0, Lp - L), (0, 0)))

    qr, kr, vr = by_residue(q), by_residue(k), by_residue(v)
    qb = qr.reshape(B, dil, H, nb, A_BLOCK, Dh)

    def band(t):
        prev = jnp.pad(t, ((0, 0), (0, 0), (0, 0), (A_BLOCK, 0), (0, 0)))[:, :, :, :Lp]
        return jnp.concatenate([prev.reshape(B, dil, H, nb, A_BLOCK, Dh),
                                t.reshape(B, dil, H, nb, A_BLOCK, Dh)], axis=4)

    kb, vb = band(kr), band(vr)
    s = jnp.einsum('brhnqd,brhnkd->brhnqk', qb, kb,
                   preferred_element_type=jnp.float32) * (Dh ** -0.5)
    qi = jnp.arange(A_BLOCK)[:, None]
    ki = jnp.arange(2 * A_BLOCK)[None, :]
    rel = qi + A_BLOCK - ki
    key_pos = jnp.arange(nb)[:, None, None] * A_BLOCK + ki[None] - A_BLOCK
    valid = (rel >= 0) & (rel <= steps) & (key_pos >= 0)
    bias = rel_bias[t5_bucket(jnp.maximum(rel, 0) * dil)].astype(jnp.float32)
    bias = bias.transpose(2, 0, 1)
    s = jnp.where(valid, s + bias[:, None], -jnp.inf)
    m = jnp.max(s, axis=-1, keepdims=True)
    p = jnp.exp(s - m)
    l = jnp.sum(p, axis=-1)
    o = jnp.einsum('brhnqk,brhnkd->brhnqd', p.astype(v.dtype), vb,
                   preferred_element_type=jnp.float32) / l[..., None]
    lse = m[..., 0] + jnp.log(l)
    o = o.reshape(B, dil, H, Lp, Dh)[:, :, :, :L].transpose(0, 3, 1, 2, 4).reshape(B, S, H, Dh)
    lse = lse.reshape(B, dil, H, Lp)[..., :L].transpose(0, 3, 1, 2).reshape(B, S, H)
    return o, lse


def dilated_attention(x, w_in, w_out, rel_bias):
    B, S, _ = x.shape
    G = len(A_GROUPS)
    qkv = (x @ w_in).reshape(B, S, G, 3, A_HEADS, A_HEAD_DIM)
    outs, lses = [], []
    for g, (window, dil) in enumerate(A_GROUPS):
        o, lse = _dilated_group(qkv[:, :, g, 0], qkv[:, :, g, 1], qkv[:, :, g, 2],
                                window, dil, rel_bias)
        outs.append(o)
        lses.append(lse)
    wts = jax.nn.softmax(jnp.stack(lses), axis=0)
    o = jnp.einsum('gbsh,gbshd->bshd', wts, jnp.stack(outs))
    return o.reshape(B, S, A_HEADS * A_HEAD_DIM).astype(x.dtype) @ w_out


def chunked_sgu(x, w_in, norm_g, norm_b, w_s, b_s, w_out):
    B, S, _ = x.shape
    z = jax.nn.gelu(x @ w_in)
    u, v = jnp.split(z, 2, axis=-1)
    v = layer_norm(v, norm_g, norm_b)
    n = S // B_CHUNK
    vc = v.reshape(B, n, B_CHUNK, B_GROUPS, B_WIDTH // B_GROUPS)
    w_causal = w_s * jnp.tril(jnp.ones((B_CHUNK, B_CHUNK), w_s.dtype))
    f = jnp.einsum('gts,bnsgc->bntgc', w_causal, vc) + b_s.T[:, :, None]
    return (u * f.reshape(B, S, B_WIDTH)) @ w_out


def causal_depthwise_conv(x, w):
    C = x.shape[-1]
    return lax.conv_general_dilated(x, w[:, None, :].astype(x.dtype), window_strides=(1,),
                                    padding=[(w.shape[0] - 1, 0)],
                                    dimension_numbers=('NWC', 'WIO', 'NWC'),
                                    feature_group_count=C)


def l2norm(t):
    t = t.astype(jnp.float32)
    return t * lax.rsqrt(jnp.sum(t * t, axis=-1, keepdims=True) + RMS_EPS)


def chunk_gated_delta_rule(q, k, v, g, beta):
    B, S, H, Dk = q.shape
    Dv = v.shape[-1]
    C = C_CHUNK
    N = S // C

    def chunks(t):
        return t.reshape(B, N, C, H, t.shape[-1]).transpose(1, 0, 3, 2, 4)

    q = chunks(q * (Dk ** -0.5))
    k = chunks(k)
    v = chunks(v)
    g = jnp.cumsum(g.reshape(B, N, C, H).transpose(1, 0, 3, 2), axis=-1)
    beta = beta.reshape(B, N, C, H).transpose(1, 0, 3, 2)
    kb = k * beta[..., None]
    vb = v * beta[..., None]
    tri = jnp.tril(jnp.ones((C, C), bool))
    strict = jnp.tril(jnp.ones((C, C), jnp.float32), -1)
    decay = jnp.exp(jnp.where(tri, g[..., :, None] - g[..., None, :], -jnp.inf))
    lower = jnp.einsum('nbhcd,nbhsd->nbhcs', kb, k) * decay * strict
    eye = jnp.eye(C, dtype=jnp.float32)
    t_inv = lax.linalg.triangular_solve(eye + lower, jnp.broadcast_to(eye, lower.shape),
                                        left_side=True, lower=True)
    w_val = t_inv @ vb
    k_cum = t_inv @ (kb * jnp.exp(g)[..., None])
    intra = jnp.einsum('nbhcd,nbhsd->nbhcs', q, k) * decay

    def step(state, xs):
        q_i, k_i, w_i, kc_i, g_i, a_i = xs
        v_new = w_i - kc_i @ state
        o = (q_i * jnp.exp(g_i)[..., None]) @ state + a_i @ v_new
        g_last = g_i[..., -1]
        state = state * jnp.exp(g_last)[..., None, None] + jnp.einsum(
            'bhcd,bhce->bhde', k_i * jnp.exp(g_last[..., None] - g_i)[..., None], v_new)
        return state, o

    state0 = jnp.zeros((B, H, Dk, Dv), jnp.float32)
    _, o = lax.scan(step, state0, (q, k, w_val, k_cum, g, intra))
    return o.transpose(1, 0, 3, 2, 4).reshape(B, S, H, Dv)


def gated_deltanet(x, w_in, conv_w, a_log, dt_bias, norm_w, w_out):
    B, S, _ = x.shape
    HD = C_HEADS * C_HEAD_DIM
    proj = x @ w_in
    qkv, z, b_raw, a_raw = jnp.split(proj, [3 * HD, 4 * HD, 4 * HD + C_HEADS], axis=-1)
    qkv = jax.nn.silu(causal_depthwise_conv(qkv, conv_w))
    q, k, v = [t.reshape(B, S, C_HEADS, C_HEAD_DIM) for t in jnp.split(qkv, 3, axis=-1)]
    q = l2norm(q)
    k = l2norm(k)
    beta = jax.nn.sigmoid(b_raw.astype(jnp.float32))
    g = -jnp.exp(a_log.astype(jnp.float32)) * jax.nn.softplus(
        a_raw.astype(jnp.float32) + dt_bias.astype(jnp.float32))
    o = chunk_gated_delta_rule(q, k, v.astype(jnp.float32), g, beta)
    o = o * lax.rsqrt(jnp.mean(o * o, axis=-1, keepdims=True) + RMS_EPS) * norm_w.astype(jnp.float32)
    o = o * jax.nn.silu(z.astype(jnp.float32).reshape(B, S, C_HEADS, C_HEAD_DIM))
    return o.reshape(B, S, HD).astype(x.dtype) @ w_out


def hierarchical_moe(x, w_coarse, w_fine, w_gate, w_up, w_down):
    B, S, D = x.shape
    xt = x.reshape(-1, D)
    T = xt.shape[0]
    coarse = jax.nn.softmax((xt @ w_coarse).astype(jnp.float32), axis=-1)
    p_grp, grp = lax.top_k(coarse, 1)
    fine_all = jnp.einsum('td,dge->tge', xt, w_fine).astype(jnp.float32)
    fine = fine_all[jnp.arange(T), grp[:, 0]]
    p_exp, idx = lax.top_k(jax.nn.softmax(fine, axis=-1), MOE_TOPK)
    gates = p_grp * p_exp / jnp.sum(p_exp, axis=-1, keepdims=True)
    expert = (grp * MOE_PER_GROUP + idx).reshape(-1)
    order = jnp.argsort(expert)
    tok = order // MOE_TOPK
    sizes = jnp.bincount(expert, length=MOE_EXPERTS).astype(jnp.int32)
    xs = xt[tok]
    h = jax.nn.silu(lax.ragged_dot(xs, w_gate, sizes)) * lax.ragged_dot(xs, w_up, sizes)
    y = lax.ragged_dot(h, w_down, sizes) * gates.reshape(-1)[order][:, None].astype(x.dtype)
    return jnp.zeros_like(xt).at[tok].add(y).reshape(B, S, D)


def setup_inputs(seed: int = 0) -> dict:
    key = jax.random.key(seed)
    ks = jax.random.split(key, 24)
    f32 = jnp.float32

    def nrm(k, shape, fan_in, scale=1.0):
        return jax.random.normal(k, shape, f32) * (scale * fan_in ** -0.5)

    a_qkv = 3 * len(A_GROUPS) * A_HEADS * A_HEAD_DIM
    hd_a = A_HEADS * A_HEAD_DIM
    hd_c = C_HEADS * C_HEAD_DIM
    c_in = 4 * hd_c + 2 * C_HEADS
    dt = jnp.exp(jax.random.uniform(ks[13], (N_C, C_HEADS), f32,
                                    minval=math.log(1e-3), maxval=math.log(0.1)))
    return {
        'x': jax.random.normal(ks[0], (BATCH, SEQ, D_MODEL), f32),
        'rel_bias': 0.1 * jax.random.normal(ks[1], (NUM_BUCKETS, A_HEADS), f32),
        'a_w_in': nrm(ks[2], (N_A, D_MODEL, a_qkv), D_MODEL),
        'a_w_out': nrm(ks[3], (N_A, hd_a, D_MODEL), hd_a, DEEPNORM_BETA),
        'b_w_in': nrm(ks[4], (N_B, D_MODEL, 2 * B_WIDTH), D_MODEL),
        'b_norm_g': 1.0 + 0.02 * jax.random.normal(ks[5], (N_B, B_WIDTH), f32),
        'b_norm_b': 0.02 * jax.random.normal(ks[6], (N_B, B_WIDTH), f32),
        'b_w_s': nrm(ks[7], (N_B, B_GROUPS, B_CHUNK, B_CHUNK), B_CHUNK),
        'b_b_s': 1.0 + 0.1 * jax.random.normal(ks[8], (N_B, B_GROUPS, B_CHUNK), f32),
        'b_w_out': nrm(ks[9], (N_B, B_WIDTH, D_MODEL), B_WIDTH, DEEPNORM_BETA),
        'c_w_in': nrm(ks[10], (N_C, D_MODEL, c_in), D_MODEL),
        'c_conv': nrm(ks[11], (N_C, C_CONV, 3 * hd_c), C_CONV),
        'c_a_log': jnp.log(jax.random.uniform(ks[12], (N_C, C_HEADS), f32, minval=1.0, maxval=16.0)),
        'c_dt_bias': dt + jnp.log(-jnp.expm1(-dt)),
        'c_norm_w': 1.0 + 0.02 * jax.random.normal(ks[14], (N_C, C_HEAD_DIM), f32),
        'c_w_out': nrm(ks[15], (N_C, hd_c, D_MODEL), hd_c, DEEPNORM_BETA),
        'ln_g': 1.0 + 0.02 * jax.random.normal(ks[16], (DEPTH, 2, D_MODEL), f32),
        'ln_b': 0.02 * jax.random.normal(ks[17], (DEPTH, 2, D_MODEL), f32),
        'moe_w_coarse': nrm(ks[18], (DEPTH, D_MODEL, MOE_GROUPS), D_MODEL),
        'moe_w_fine': nrm(ks[19], (DEPTH, D_MODEL, MOE_GROUPS, MOE_PER_GROUP), D_MODEL),
        'moe_w_gate': nrm(ks[20], (DEPTH, MOE_EXPERTS, D_MODEL, MOE_HIDDEN), D_MODEL),
        'moe_w_up': nrm(ks[21], (DEPTH, MOE_EXPERTS, D_MODEL, MOE_HIDDEN), D_MODEL),
        'moe_w_down': nrm(ks[22], (DEPTH, MOE_EXPERTS, MOE_HIDDEN, D_MODEL), MOE_HIDDEN, DEEPNORM_BETA),
    }


def reference(x, rel_bias, a_w_in, a_w_out, b_w_in, b_norm_g, b_norm_b, b_w_s, b_b_s, b_w_out,
              c_w_in, c_conv, c_a_log, c_dt_bias, c_norm_w, c_w_out, ln_g, ln_b,
              moe_w_coarse, moe_w_fine, moe_w_gate, moe_w_up, moe_w_down):
    for i in range(DEPTH):
        kind, j = i % N_MIXERS, i // N_MIXERS
        if kind == 0:
            h = dilated_attention(x, a_w_in[j], a_w_out[j], rel_bias)
        elif kind == 1:
            h = chunked_sgu(x, b_w_in[j], b_norm_g[j], b_norm_b[j], b_w_s[j], b_b_s[j], b_w_out[j])
        else:
            h = gated_deltanet(x, c_w_in[j], c_conv[j], c_a_log[j], c_dt_bias[j], c_norm_w[j], c_w_out[j])
        x = layer_norm(DEEPNORM_ALPHA * x + h, ln_g[i, 0], ln_b[i, 0])
        h = hierarchical_moe(x, moe_w_coarse[i], moe_w_fine[i], moe_w_gate[i], moe_w_up[i], moe_w_down[i])
        x = layer_norm(DEEPNORM_ALPHA * x + h, ln_g[i, 1], ln_b[i, 1])
    return x
```

```python
import numpy as np
import ml_dtypes
import concourse.bass as bass
import concourse.mybir as mybir
from concourse.bass_utils import run_bass_kernel_spmd

F32 = mybir.dt.float32
BF16 = mybir.dt.bfloat16
I32 = mybir.dt.int32
U32 = mybir.dt.uint32
AF = mybir.ActivationFunctionType
ALU = mybir.AluOpType
AX = mybir.AxisListType

N_DMA_SEMS = 24


class T:
    def __init__(self, h, name):
        self.h = h
        self.name = name
        self.excl = 'psum' in str(getattr(h, 'space', '')).lower() or 'psum' in str(type(h)).lower()
        self.w = None
        self.r = {}

    def __getitem__(self, idx):
        return self.h[idx]


class KB:
    def __init__(self, nc, same_engine_sync=True):
        self.nc = nc
        self.eng = dict(pe=nc.tensor, dve=nc.vector, act=nc.scalar, pool=nc.gpsimd, sp=nc.sync)
        self.sem = {e: nc.alloc_semaphore("s_" + e) for e in self.eng}
        self.cnt = {e: 0 for e in self.eng}
        self.waited = {e: {} for e in self.eng}
        self.dsem = [nc.alloc_semaphore("s_dma%d" % i) for i in range(N_DMA_SEMS)]
        self.dcnt = [0] * N_DMA_SEMS
        self.drr = 0
        self.same_engine_sync = same_engine_sync
        self.n_inst = 0
        self.n_wait = 0
        self.out_marks = []

    def sb(self, name, shape, dtype=F32):
        return T(self.nc.alloc_sbuf_tensor(name, list(shape), dtype), name)

    def ps(self, name, shape, dtype=F32):
        return T(self.nc.alloc_psum_tensor(name, list(shape), dtype), name)

    def dram(self, name, shape, dtype, kind):
        return T(self.nc.dram_tensor(name, list(shape), dtype, kind=kind), name)

    def _semh(self, key):
        return self.sem[key] if isinstance(key, str) else self.dsem[key]

    def _wait(self, e, key, val):
        if key == e and (e == 'pe' or not self.same_engine_sync):
            return
        if self.waited[e].get(key, 0) >= val:
            return
        self.eng[e].wait_ge(self._semh(key), val)
        self.waited[e][key] = val
        self.n_wait += 1

    def _deps(self, e, reads, writes):
        deps = {}

        def add(m):
            if m is None:
                return
            k, v = m
            if deps.get(k, 0) < v:
                deps[k] = v
        for t in reads:
            add(t.w)
            if t.excl:
                for k, v in t.r.items():
                    if k != e:
                        add((k, v))
        for t in writes:
            add(t.w)
            for k, v in t.r.items():
                add((k, v))
        for k, v in deps.items():
            self._wait(e, k, v)

    def _mark(self, mark, reads, writes):
        k, v = mark
        for t in reads:
            if t.r.get(k, 0) < v:
                t.r[k] = v
        for t in writes:
            t.w = mark
            t.r = {}

    def op(self, e, fn, reads=(), writes=(), ser=False):
        self._deps(e, reads, writes)
        if ser and self.cnt[e] and self.waited[e].get(e, 0) < self.cnt[e]:
            self.eng[e].wait_ge(self.sem[e], self.cnt[e])
            self.waited[e][e] = self.cnt[e]
        inst = fn(self.eng[e])
        self.cnt[e] += 1
        inst.then_inc(self.sem[e], 1)
        self.n_inst += 1
        self._mark((e, self.cnt[e]), reads, writes)
        return inst

    def mm(self, out_ap, lhsT, rhs, start, stop, reads=(), writes=(), **kw):
        return self.op('pe', lambda en: en.matmul(out_ap, lhsT, rhs, start=start, stop=stop, **kw),
                       reads=reads, writes=writes)

    def dma(self, q, out_ap, in_ap, reads=(), writes=(), **kw):
        self._deps(q, reads, writes)
        i = self.drr
        self.drr = (self.drr + 1) % N_DMA_SEMS
        self._wait(q, i, self.dcnt[i])
        inst = self.eng[q].dma_start(out=out_ap, in_=in_ap, **kw)
        self.dcnt[i] += 16
        inst.then_inc(self.dsem[i], 16)
        self.n_inst += 1
        self._mark((i, self.dcnt[i]), reads, writes)
        return (i, self.dcnt[i])

    def barrier(self):
        for e in ('pe', 'dve', 'act', 'pool', 'sp'):
            for i in range(N_DMA_SEMS):
                if self.dcnt[i]:
                    self._wait(e, i, self.dcnt[i])
            for o in ('pe', 'dve', 'act', 'pool'):
                if o != e and self.cnt[o]:
                    self._wait(e, o, self.cnt[o])

    def finish(self, out_tiles):
        for i in range(N_DMA_SEMS):
            self._wait('sp', i, self.dcnt[i])
        for e in ('pe', 'dve', 'act', 'pool'):
            if self.cnt[e]:
                self._wait('sp', e, self.cnt[e])

import contextlib
import math

TOK = 2048
D = 1024
NT = TOK // 128
ALPHA = (2 * 4) ** 0.25
LN_EPS = 1e-5
NCORES = 8


def _scope_patch():
    def scope(self):
        @contextlib.contextmanager
        def cm():
            st = contextlib.ExitStack()
            self._stacks.append(st)
            try:
                with st:
                    yield
                    self.barrier()
            finally:
                self._stacks.pop()
        return cm()

    def sb(self, name, shape, dtype=F32):
        self._uid = getattr(self, '_uid', 0) + 1
        name = "%s_%d" % (name, self._uid)
        if getattr(self, '_stacks', None):
            h = self._stacks[-1].enter_context(self.nc.sbuf_tensor(name, list(shape), dtype))
        else:
            h = self.nc.alloc_sbuf_tensor(name, list(shape), dtype)
        return T(h, name)

    def ps(self, name, shape, dtype=F32):
        self._uid = getattr(self, '_uid', 0) + 1
        name = "%s_%d" % (name, self._uid)
        if getattr(self, '_stacks', None):
            h = self._stacks[-1].enter_context(self.nc.psum_tensor(name, list(shape), dtype))
        else:
            h = self.nc.alloc_psum_tensor(name, list(shape), dtype)
        return T(h, name)
    KB.scope = scope
    KB.sb = sb
    KB.ps = ps


_scope_patch()


def new_kb():
    nc = bass.Bass("TRN2", target_bir_lowering=False)
    k = KB(nc, same_engine_sync=True)
    k._stacks = []
    return nc, k


class Stager:
    def __init__(self, k, n=2, width=4096):
        self.k = k
        self.width = width
        self.bufs = [k.sb("stage%d" % i, [128, width]) for i in range(n)]
        self.i = 0

    def load(self, dst_t, dst_ap, src_ap, eng='pool'):
        k = self.k
        shp = list(dst_ap.shape)
        if len(shp) == 2:
            dst_ap = dst_ap.unsqueeze(1)
            src_ap = src_ap.unsqueeze(1)
            shp = list(dst_ap.shape)
        p, a, b = shp
        assert b <= self.width
        step = max(1, self.width // b)
        for a0 in range(0, a, step):
            a1 = min(a, a0 + step)
            st = self.bufs[self.i % len(self.bufs)]
            q = 'sp' if self.i % 2 == 0 else 'act'
            self.i += 1
            sv = st[0:p, 0:(a1 - a0) * b].rearrange("p (a b) -> p a b", b=b)
            k.dma(q, sv, src_ap[:, a0:a1, :], writes=[st])
            k.op(eng, lambda en: en.tensor_copy(dst_ap[:, a0:a1, :], sv), reads=[st], writes=[dst_t])


def ln_rows(k, ya, yt, W, gb, bb, gbt, bbt, st, eng2='pool', eps=LN_EPS):
    nch = W // 512
    for c in range(nch):
        k.op('dve', lambda e: e.bn_stats(st[:, 6 * c:6 * c + 6], ya[:, c * 512:(c + 1) * 512]), reads=[yt], writes=[st])
    mv = st[:, 48:50]
    rs = st[:, 50:51]
    k.op('dve', lambda e: e.bn_aggr(mv, st[:, 0:6 * nch]), reads=[st], writes=[st])
    k.op('dve', lambda e: e.tensor_scalar(rs, st[:, 49:50], eps, None, ALU.add), reads=[st], writes=[st])
    k.op('act', lambda e: e.activation(rs, rs, AF.Sqrt), reads=[st], writes=[st])
    k.op('dve', lambda e: e.reciprocal(rs, rs), reads=[st], writes=[st])
    k.op('dve', lambda e: e.tensor_scalar(ya, ya, st[:, 48:49], rs, ALU.subtract, ALU.mult), reads=[yt, st], writes=[yt])
    k.op(eng2, lambda e: e.tensor_tensor(ya, ya, gb, ALU.mult), reads=[yt, gbt], writes=[yt])
    k.op(eng2, lambda e: e.tensor_tensor(ya, ya, bb, ALU.add), reads=[yt, bbt], writes=[yt])


def post(k, x_src, h_src, lng, lnb, wr, idn, x1_out, x1T_out, gt_out):
    with k.scope():
        ident = k.sb("p_ident", [128, 128])
        wrt = k.sb("p_wrt", [128, 8, 72])
        gb = k.sb("p_gb", [128, D]); bb = k.sb("p_bb", [128, D])
        xt = [k.sb("p_x%d" % i, [128, D]) for i in range(2)]
        ht = [k.sb("p_h%d" % i, [128, D]) for i in range(2)]
        xT32 = [k.sb("p_xT32_%d" % i, [128, 8, 128]) for i in range(2)]
        xTb = [k.sb("p_xTb%d" % i, [128, 8, 128], BF16) for i in range(2)]
        st = [k.sb("p_st%d" % i, [128, 64]) for i in range(2)]
        L = k.sb("p_L", [128, NT, 72])
        GT = k.sb("p_GT", [64, TOK])
        R = k.sb("p_R", [128, 4096])
        pT = [k.ps("p_pT%d" % i, [128, 512]) for i in range(4)]
        pL = [k.ps("p_pL%d" % i, [128, 512]) for i in range(2)]
        k.dma('sp', ident[:], idn[:], writes=[ident])
        k.dma('sp', wrt[:], wr[:].rearrange("(kc p) n -> p kc n", p=128), writes=[wrt])
        k.dma('sp', gb[:], lng[:].partition_broadcast(128), writes=[gb])
        k.dma('act', bb[:], lnb[:].partition_broadcast(128), writes=[bb])
        for j in range(NT):
            x_ = xt[j % 2]; h_ = ht[j % 2]
            k.dma('sp', x_[:], x_src[j * 128:(j + 1) * 128, :], writes=[x_])
            k.dma('act', h_[:], h_src[j * 128:(j + 1) * 128, :], reads=[h_src] if isinstance(h_src, T) else [], writes=[h_])
            k.op('dve', lambda e: e.scalar_tensor_tensor(x_[:], x_[:], ALPHA, h_[:], ALU.mult, ALU.add), reads=[x_, h_], writes=[x_])
            ln_rows(k, x_[:], x_, D, gb[:], bb[:], gb, bb, st[j % 2])
            k.dma('sp', x1_out[j * 128:(j + 1) * 128, :], x_[:], reads=[x_], writes=[])
            x32 = xT32[j % 2]; xb = xTb[j % 2]
            for hf in range(2):
                pt = pT[(2 * j + hf) % 4]
                for kk in range(4):
                    kc = hf * 4 + kk
                    k.op('pe', lambda e: e.transpose(pt[:, kk * 128:(kk + 1) * 128], x_[:, kc * 128:(kc + 1) * 128], ident[:]),
                         reads=[x_, ident], writes=[pt])
                k.op('act', lambda e: e.copy(x32[:, hf * 4:hf * 4 + 4, :], pt[:].rearrange("p (k t) -> p k t", k=4)), reads=[pt], writes=[x32])
            k.op('pool', lambda e: e.tensor_copy(xb[:], x32[:]), reads=[x32], writes=[xb])
            k.dma('act', x1T_out[:, j * 128:(j + 1) * 128].rearrange("(kc p) t -> p kc t", p=128), xb[:], reads=[xb], writes=[])
            pl = pL[j % 2]
            for kc in range(8):
                k.mm(pl[:, 0:72], x32[:, kc, :], wrt[:, kc, :], kc == 0, kc == 7, reads=[x32, wrt], writes=[pl])
            k.op('act', lambda e: e.copy(L[:, j, :], pl[:, 0:72]), reads=[pl], writes=[L])
        off = [0]

        def rt(n):
            a = R[:, off[0]:off[0] + n]
            off[0] += n
            return a

        def v3(a):
            return a.rearrange("p (j g) -> p j g", j=NT)
        cm = rt(NT); cs = v3(rt(NT * 8)); ohg = v3(rt(NT * 8)); ce = v3(rt(NT * 8)); csum = rt(NT); pgrp = rt(NT)
        fm = rt(NT * 64).rearrange("p (j g e) -> p j g e", j=NT, g=8)
        fsel = v3(rt(NT * 8)); m2 = rt(NT); fs = v3(rt(NT * 8)); mk1 = v3(rt(NT * 8)); fs2 = v3(rt(NT * 8))
        m3 = rt(NT); mk2 = v3(rt(NT * 8)); eb = rt(NT); den = rt(NT); g1 = rt(NT); g2 = rt(NT)
        t1 = v3(rt(NT * 8)); t2 = v3(rt(NT * 8)); Gf = v3(rt(NT * 8))
        G = rt(NT * 64).rearrange("p (j g e) -> p j g e", j=NT, g=8)

        def bc3(a):
            return a.unsqueeze(2).to_broadcast([128, NT, 8])

        def r(eng, fn):
            k.op(eng, fn, reads=[R, L], writes=[R])
        Lc = L[:, :, 0:8]
        Lf = L[:, :, 8:72].rearrange("p j (g e) -> p j g e", g=8)
        ohg4 = ohg.unsqueeze(3).to_broadcast([128, NT, 8, 8])
        r('dve', lambda e: e.tensor_reduce(cm, Lc, AX.X, ALU.max))
        r('dve', lambda e: e.tensor_tensor(cs, Lc, bc3(cm), ALU.subtract))
        r('dve', lambda e: e.tensor_single_scalar(ohg, cs, 0.0, ALU.is_equal))
        r('act', lambda e: e.activation(ce, cs, AF.Exp))
        r('dve', lambda e: e.tensor_reduce(csum, ce, AX.X, ALU.add))
        r('dve', lambda e: e.reciprocal(pgrp, csum))
        r('dve', lambda e: e.tensor_tensor(fm, Lf, ohg4, ALU.mult))
        r('dve', lambda e: e.tensor_reduce(fsel, fm.rearrange("p j g e -> p j e g"), AX.X, ALU.add))
        r('dve', lambda e: e.tensor_reduce(m2, fsel, AX.X, ALU.max))
        r('dve', lambda e: e.tensor_tensor(fs, fsel, bc3(m2), ALU.subtract))
        r('dve', lambda e: e.tensor_single_scalar(mk1, fs, 0.0, ALU.is_equal))
        r('dve', lambda e: e.scalar_tensor_tensor(fs2, mk1, -1e30, fs, ALU.mult, ALU.add))
        r('dve', lambda e: e.tensor_reduce(m3, fs2, AX.X, ALU.max))
        r('dve', lambda e: e.tensor_tensor(mk2, fs2, bc3(m3), ALU.is_equal))
        r('act', lambda e: e.activation(eb, m3, AF.Exp))
        r('dve', lambda e: e.tensor_scalar(den, eb, 1.0, None, ALU.add))
        r('dve', lambda e: e.reciprocal(den, den))
        r('dve', lambda e: e.tensor_tensor(g1, pgrp, den, ALU.mult))
        r('dve', lambda e: e.tensor_tensor(g2, g1, eb, ALU.mult))
        r('dve', lambda e: e.tensor_tensor(t1, mk1, bc3(g1), ALU.mult))
        r('dve', lambda e: e.tensor_tensor(t2, mk2, bc3(g2), ALU.mult))
        r('dve', lambda e: e.tensor_tensor(Gf, t1, t2, ALU.add))
        r('dve', lambda e: e.tensor_tensor(G, ohg4, Gf.unsqueeze(2).to_broadcast([128, NT, 8, 8]), ALU.mult))
        for j4 in range(NT // 4):
            pt = pT[j4 % 4]
            for jj in range(4):
                j = j4 * 4 + jj
                k.op('pe', lambda e: e.transpose(pt[0:64, jj * 128:(jj + 1) * 128],
                                                  G[:, j, :, :].rearrange("p g e -> p (g e)"), ident[:]),
                     reads=[R, ident], writes=[pt])
            k.op('act', lambda e: e.copy(GT[:, j4 * 512:(j4 + 1) * 512], pt[0:64, 0:512]), reads=[pt], writes=[GT])
        k.dma('sp', gt_out[:, :], GT[:], reads=[GT], writes=[])


NTOKALL = 16384
HID = 512
EPC = 8


def build_E(n_blocks=NTOKALL // TOK, n_exp=EPC):
    nc, k = new_kb()
    ntok = n_blocks * TOK
    xT = k.dram("xT", [D, ntok], BF16, "ExternalInput")
    gt = k.dram("gt", [EPC, ntok], F32, "ExternalInput")
    wg = k.dram("wg", [EPC, D, HID], F32, "ExternalInput")
    wu = k.dram("wu", [EPC, D, HID], F32, "ExternalInput")
    wd = k.dram("wd", [EPC, HID, D], F32, "ExternalInput")
    idn = k.dram("idn", [128, 128], F32, "ExternalInput")
    yp = k.dram("yp", [ntok, D], BF16, "ExternalOutput")
    ident = k.sb("ident", [128, 128])
    k.dma('sp', ident[:], idn[:], writes=[ident])
    yacc_h = nc.alloc_sbuf_tensor("yacc", [128, NT, D], F32)
    yacc = [[T(yacc_h, "yacc%d_%d" % (j, h)) for h in range(2)] for j in range(NT)]
    xTb = k.sb("xTb", [128, 8, TOK], BF16)
    GTb = k.sb("GTb", [EPC, TOK])
    Wg = [k.sb("Wg%d" % i, [128, 8, HID], BF16) for i in range(2)]
    Wu = [k.sb("Wu%d" % i, [128, 8, HID], BF16) for i in range(2)]
    Wd = [k.sb("Wd%d" % i, [128, 4, D], BF16) for i in range(2)]
    stg = Stager(k, 2, 4096)
    hT = [k.sb("hT%d" % i, [128, 4, 512], BF16) for i in range(2)]
    sg = [k.sb("sg%d" % i, [128, 512], BF16) for i in range(2)]
    tt_ = [k.sb("tt%d" % i, [128, 512], BF16) for i in range(2)]
    yo = [k.sb("yo%d" % i, [128, D], BF16) for i in range(2)]
    pg = [k.ps("pg%d" % i, [128, 512]) for i in range(2)]
    pu = [k.ps("pu%d" % i, [128, 512]) for i in range(2)]
    pb = [k.ps("pb%d" % i, [128, 512]) for i in range(2)]
    py = [k.ps("py%d" % i, [128, 512]) for i in range(2)]
    cnt = dict(hc=0, ett=0, py=0, w=0)
    pending = [None]

    def load_w(e, slot):
        stg.load(Wg[slot], Wg[slot][:], wg[e].rearrange("(kc p) h -> p kc h", p=128))
        stg.load(Wu[slot], Wu[slot][:], wu[e].rearrange("(kc p) h -> p kc h", p=128))
        stg.load(Wd[slot], Wd[slot][:], wd[e].rearrange("(hc p) d -> p hc d", p=128))

    def down(slot, tt, hbuf, first):
        for ts in range(4):
            for dh in range(2):
                pyt = py[cnt['py'] % 2]
                cnt['py'] += 1
                for hc in range(4):
                    k.mm(pyt[:], hbuf[:, hc, ts * 128:(ts + 1) * 128], Wd[slot][:, hc, dh * 512:(dh + 1) * 512],
                         hc == 0, hc == 3, reads=[hbuf, Wd[slot]], writes=[pyt])
                j = tt * 4 + ts
                ya = yacc_h[:, j, dh * 512:(dh + 1) * 512]
                if first:
                    k.op('dve', lambda en: en.tensor_copy(ya, pyt[:]), reads=[pyt], writes=[yacc[j][dh]])
                else:
                    k.op('dve', lambda en: en.tensor_tensor(ya, ya, pyt[:], ALU.add), reads=[pyt, yacc[j][dh]], writes=[yacc[j][dh]])

    def flush():
        if pending[0] is not None:
            pending[0]()
            pending[0] = None

    seq = [(tb, e) for tb in range(n_blocks) for e in range(n_exp)]
    load_w(seq[0][1], 0)
    for si, (tb, e) in enumerate(seq):
        slot = si % 2
        if e == 0:
            flush()
            if tb > 0:
                store_block(k, yacc_h, yacc, yo, yp, tb - 1)
            k.dma('sp', xTb[:], xT[:, tb * TOK:(tb + 1) * TOK].rearrange("(kc p) t -> p kc t", p=128), writes=[xTb])
            k.dma('act', GTb[:], gt[:, tb * TOK:(tb + 1) * TOK], writes=[GTb])
        for tt in range(4):
            pbt = pb[cnt['ett'] % 2]
            hT_t = hT[cnt['ett'] % 2]
            cnt['ett'] += 1
            k.mm(pbt[:], ident[0:EPC, e:e + 1].to_broadcast([EPC, 128]), GTb[:, tt * 512:(tt + 1) * 512], True, True,
                 reads=[ident, GTb], writes=[pbt])
            for hc in range(4):
                b = cnt['hc'] % 2
                cnt['hc'] += 1
                for kc in range(8):
                    k.mm(pg[b][:], Wg[slot][:, kc, hc * 128:(hc + 1) * 128], xTb[:, kc, tt * 512:(tt + 1) * 512],
                         kc == 0, kc == 7, reads=[Wg[slot], xTb], writes=[pg[b]])
                for kc in range(8):
                    k.mm(pu[b][:], Wu[slot][:, kc, hc * 128:(hc + 1) * 128], xTb[:, kc, tt * 512:(tt + 1) * 512],
                         kc == 0, kc == 7, reads=[Wu[slot], xTb], writes=[pu[b]])
                k.op('act', lambda en: en.activation(sg[b][:], pg[b][:], AF.Silu), reads=[pg[b]], writes=[sg[b]])
                k.op('dve', lambda en: en.tensor_tensor(tt_[b][:], sg[b][:], pu[b][:], ALU.mult), reads=[sg[b], pu[b]], writes=[tt_[b]])
                k.op('dve', lambda en: en.tensor_tensor(hT_t[:, hc, :], tt_[b][:], pbt[:], ALU.mult),
                     reads=[tt_[b], pbt], writes=[hT_t])
            flush()
            if tt == 0 and si + 1 < len(seq):
                load_w(seq[si + 1][1], 1 - slot)
            pending[0] = (lambda slot=slot, tt=tt, hT_t=hT_t, first=(e == 0): down(slot, tt, hT_t, first))
    flush()
    store_block(k, yacc_h, yacc, yo, yp, n_blocks - 1)
    k.finish([])
    return nc


def store_block(k, yacc_h, yacc, yo, yp, tb):
    for j in range(NT):
        o = yo[j % 2]
        k.op('act' if j % 2 == 0 else 'pool', lambda en: (en.copy if j % 2 == 0 else en.tensor_copy)(o[:], yacc_h[:, j, :]),
             reads=[yacc[j][0], yacc[j][1]], writes=[o])
        k.dma('sp' if j % 2 == 0 else 'act', yp[tb * TOK + j * 128: tb * TOK + (j + 1) * 128, :], o[:], reads=[o], writes=[])


def build_C():
    nc, k = new_kb()
    x1 = k.dram("x1", [TOK, D], F32, "ExternalInput")
    yp = k.dram("yp", [NCORES, TOK, D], BF16, "ExternalInput")
    lng = k.dram("lng", [D], F32, "ExternalInput")
    lnb = k.dram("lnb", [D], F32, "ExternalInput")
    out = k.dram("out", [TOK, D], F32, "ExternalOutput")
    gb = k.sb("gb", [128, D]); bb = k.sb("bb", [128, D])
    k.dma('sp', gb[:], lng[:].partition_broadcast(128), writes=[gb])
    k.dma('act', bb[:], lnb[:].partition_broadcast(128), writes=[bb])
    xt = [k.sb("c_x%d" % i, [128, D]) for i in range(2)]
    yt = [k.sb("c_y%d" % i, [128, NCORES, D], BF16) for i in range(2)]
    st = [k.sb("st%d" % i, [128, 64]) for i in range(2)]
    for j in range(NT):
        x_ = xt[j % 2]; y_ = yt[j % 2]
        k.dma('sp', x_[:], x1[j * 128:(j + 1) * 128, :], writes=[x_])
        k.dma('act', y_[:], yp[:, j * 128:(j + 1) * 128, :].rearrange("c p d -> p c d"), writes=[y_])
        k.op('dve', lambda e: e.scalar_tensor_tensor(x_[:], x_[:], ALPHA, y_[:, 0, :], ALU.mult, ALU.add), reads=[x_, y_], writes=[x_])
        for c in range(1, NCORES):
            k.op('dve' if c % 2 else 'pool', lambda e: e.tensor_tensor(x_[:], x_[:], y_[:, c, :], ALU.add), reads=[x_, y_], writes=[x_])
        ln_rows(k, x_[:], x_, D, gb[:], bb[:], gb, bb, st[j % 2])
        k.dma('sp', out[j * 128:(j + 1) * 128, :], x_[:], reads=[x_], writes=[])
    k.finish([])
    return nc


GELU_C = 1.5957691216057308


def gelu_tanh(k, out_ap, out_t, ps_t, ps_ap, tmp):
    xs, x2, z = tmp
    k.op('act', lambda e: e.copy(xs[:], ps_ap), reads=[ps_t], writes=[xs])
    k.op('dve', lambda e: e.tensor_tensor(x2[:], xs[:], xs[:], ALU.mult), reads=[xs], writes=[x2])
    k.op('dve', lambda e: e.tensor_scalar(x2[:], x2[:], 0.044715, 1.0, ALU.mult, ALU.add), reads=[x2], writes=[x2])
    k.op('pool', lambda e: e.tensor_tensor(z[:], x2[:], xs[:], ALU.mult), reads=[x2, xs], writes=[z])
    k.op('act', lambda e: e.activation(z[:], z[:], AF.Sigmoid, scale=GELU_C), reads=[z], writes=[z])
    k.op('dve', lambda e: e.tensor_tensor(out_ap, xs[:], z[:], ALU.mult), reads=[xs, z], writes=[out_t])


def load_xT(k, xT, x_src, ident, n_tiles, pT, tok0=0):
    with k.scope():
        xt = [k.sb("lx%d" % i, [128, D]) for i in range(2)]
        for j in range(n_tiles):
            x_ = xt[j % 2]
            k.dma('sp' if j % 2 == 0 else 'act', x_[:], x_src[j * 128:(j + 1) * 128, :], writes=[x_])
            for hf in range(2):
                pt = pT[(2 * j + hf) % len(pT)]
                for kk in range(4):
                    kc = hf * 4 + kk
                    k.op('pe', lambda e: e.transpose(pt[:, kk * 128:(kk + 1) * 128], x_[:, kc * 128:(kc + 1) * 128], ident[:]),
                         reads=[x_, ident], writes=[pt])
                k.op('act' if hf == 0 else 'dve',
                     lambda e: (e.copy if hf == 0 else e.tensor_copy)(xT[:, hf * 4:hf * 4 + 4, tok0 + j * 128:tok0 + (j + 1) * 128],
                                                                     pt[:].rearrange("p (k t) -> p k t", k=4)),
                     reads=[pt], writes=[xT])


def declare_post_io(k, fused=True):
    nc = k.nc
    io = dict(
        lng=k.dram("lng", [D], F32, "ExternalInput"), lnb=k.dram("lnb", [D], F32, "ExternalInput"),
        wr=k.dram("wr", [D, 72], F32, "ExternalInput"), idn=k.dram("idn", [128, 128], F32, "ExternalInput"))
    if not fused:
        io.update(x1o=k.dram("x1o", [TOK, D], F32, "ExternalOutput"), x1T=k.dram("x1T", [D, TOK], BF16, "ExternalOutput"),
                  gto=k.dram("gto", [64, TOK], F32, "ExternalOutput"))
    else:
        io.update(x1o=T(nc.dram_tensor("x1o_scr", [TOK, D], F32), "x1o_scr"), x1T=T(nc.dram_tensor("x1T_scr", [D, TOK], BF16), "x1T_scr"),
                  gto=T(nc.dram_tensor("gt_scr", [64, TOK], F32), "gt_scr"),
                  wg=k.dram("wg", [64, D, HID], F32, "ExternalInput"), wu=k.dram("wu", [64, D, HID], F32, "ExternalInput"),
                  wd=k.dram("wd", [64, HID, D], F32, "ExternalInput"),
                  lng2=k.dram("lng2", [D], F32, "ExternalInput"), lnb2=k.dram("lnb2", [D], F32, "ExternalInput"),
                  x2=k.dram("x2", [TOK, D], F32, "ExternalOutput"))
    io['fused'] = fused
    return io


def tail(k, io):
    if io['fused']:
        moe_tp(k, io)
    k.finish([])
    return k.nc


def moe_tp(k, io, n_exp=64):
    nc = k.nc
    wg, wu, wd = io['wg'], io['wu'], io['wd']
    with k.scope():
        ident = k.sb("e_ident", [128, 128])
        k.dma('sp', ident[:], io['idn'][:], writes=[ident])
        yacc_t = k.sb("e_yacc", [128, NT, D])
        yacc_h = yacc_t.h
        yacc = [[T(yacc_h, "yacc%d_%d" % (j, h)) for h in range(2)] for j in range(NT)]
        yfull = [T(yacc_h, "yaccf%d" % j) for j in range(NT)]
        xTb = k.sb("e_xTb", [128, 8, TOK], BF16)
        GTb = k.sb("e_GTb", [64, TOK])
        Wg = [k.sb("e_Wg%d" % i, [128, 8, HID], BF16) for i in range(2)]
        Wu = [k.sb("e_Wu%d" % i, [128, 8, HID], BF16) for i in range(2)]
        Wd = [k.sb("e_Wd%d" % i, [128, 4, D], BF16) for i in range(2)]
        stg = Stager(k, 2, 4096)
        hT = [k.sb("e_hT%d" % i, [128, 4, 512], BF16) for i in range(2)]
        sg = [k.sb("e_sg%d" % i, [128, 512], BF16) for i in range(2)]
        tt_ = [k.sb("e_tt%d" % i, [128, 512], BF16) for i in range(2)]
        pg = [k.ps("e_pg%d" % i, [128, 512]) for i in range(2)]
        pu = [k.ps("e_pu%d" % i, [128, 512]) for i in range(2)]
        pb = [k.ps("e_pb%d" % i, [128, 512]) for i in range(2)]
        py = [k.ps("e_py%d" % i, [128, 512]) for i in range(2)]
        cnt = dict(hc=0, ett=0, py=0)
        pending = [None]
        k.dma('sp', xTb[:], io['x1T'][:, :].rearrange("(kc p) t -> p kc t", p=128), writes=[xTb])
        k.dma('act', GTb[:], io['gto'][:, :], writes=[GTb])
        for j in range(NT):
            k.dma('sp' if j % 2 == 0 else 'act', yacc_h[:, j, :], io['x1o'][j * 128:(j + 1) * 128, :], writes=[yfull[j]])
            k.op('pool', lambda e: e.tensor_scalar(yacc_h[:, j, :], yacc_h[:, j, :], ALPHA, None, ALU.mult), reads=[yfull[j]], writes=[yfull[j]])
        k.barrier()

        def load_w(e, slot):
            stg.load(Wg[slot], Wg[slot][:], wg[e].rearrange("(kc p) h -> p kc h", p=128))
            stg.load(Wu[slot], Wu[slot][:], wu[e].rearrange("(kc p) h -> p kc h", p=128))
            stg.load(Wd[slot], Wd[slot][:], wd[e].rearrange("(hc p) d -> p hc d", p=128))

        def down(slot, tt, hbuf):
            for ts in range(4):
                for dh in range(2):
                    pyt = py[cnt['py'] % 2]
                    cnt['py'] += 1
                    for hc in range(4):
                        k.mm(pyt[:], hbuf[:, hc, ts * 128:(ts + 1) * 128], Wd[slot][:, hc, dh * 512:(dh + 1) * 512],
                             hc == 0, hc == 3, reads=[hbuf, Wd[slot]], writes=[pyt])
                    j = tt * 4 + ts
                    ya = yacc_h[:, j, dh * 512:(dh + 1) * 512]
                    k.op('dve', lambda en: en.tensor_tensor(ya, ya, pyt[:], ALU.add), reads=[pyt, yacc[j][dh]], writes=[yacc[j][dh]])

        def flush():
            if pending[0] is not None:
                pending[0]()
                pending[0] = None

        load_w(0, 0)
        for e in range(n_exp):
            slot = e % 2
            for tt in range(4):
                pbt = pb[cnt['ett'] % 2]
                hT_t = hT[cnt['ett'] % 2]
                cnt['ett'] += 1
                k.mm(pbt[:], ident[0:64, e:e + 1].to_broadcast([64, 128]), GTb[:, tt * 512:(tt + 1) * 512], True, True,
                     reads=[ident, GTb], writes=[pbt])
                for hc in range(4):
                    b = cnt['hc'] % 2
                    cnt['hc'] += 1
                    for kc in range(8):
                        k.mm(pg[b][:], Wg[slot][:, kc, hc * 128:(hc + 1) * 128], xTb[:, kc, tt * 512:(tt + 1) * 512],
                             kc == 0, kc == 7, reads=[Wg[slot], xTb], writes=[pg[b]])
                    for kc in range(8):
                        k.mm(pu[b][:], Wu[slot][:, kc, hc * 128:(hc + 1) * 128], xTb[:, kc, tt * 512:(tt + 1) * 512],
                             kc == 0, kc == 7, reads=[Wu[slot], xTb], writes=[pu[b]])
                    k.op('act', lambda en: en.activation(sg[b][:], pg[b][:], AF.Silu), reads=[pg[b]], writes=[sg[b]])
                    k.op('dve', lambda en: en.tensor_tensor(tt_[b][:], sg[b][:], pu[b][:], ALU.mult), reads=[sg[b], pu[b]], writes=[tt_[b]])
                    k.op('dve', lambda en: en.tensor_tensor(hT_t[:, hc, :], tt_[b][:], pbt[:], ALU.mult),
                         reads=[tt_[b], pbt], writes=[hT_t])
                flush()
                if tt == 0 and e + 1 < n_exp:
                    load_w(e + 1, 1 - slot)
                pending[0] = (lambda slot=slot, tt=tt, hT_t=hT_t: down(slot, tt, hT_t))
        flush()
        k.barrier()
        gbf = Wg[0]; bbf = Wu[0]
        gbv = gbf.h[:].rearrange("p k h -> p (k h)").bitcast(F32)[:, 0:D]
        bbv = bbf.h[:].rearrange("p k h -> p (k h)").bitcast(F32)[:, 0:D]
        k.dma('sp', gbv, io['lng2'][:].partition_broadcast(128), writes=[gbf])
        k.dma('act', bbv, io['lnb2'][:].partition_broadcast(128), writes=[bbf])
        st = [k.sb("e_st%d" % i, [128, 64]) for i in range(2)]
        for j in range(NT):
            ln_rows(k, yacc_h[:, j, :], yfull[j], D, gbv, bbv, gbf, bbf, st[j % 2])
            k.dma('sp' if j % 2 == 0 else 'act', io['x2'][j * 128:(j + 1) * 128, :], yacc_h[:, j, :], reads=[yfull[j]], writes=[])


def build_M_sgu(n_chunks=NT, fused=True):
    nc, k = new_kb()
    x = k.dram("x", [TOK, D], F32, "ExternalInput")
    w_in = k.dram("w_in", [D, 4096], F32, "ExternalInput")
    ng = k.dram("ng", [2048], F32, "ExternalInput")
    nb = k.dram("nb", [2048], F32, "ExternalInput")
    w_s = k.dram("w_s", [16, 128, 128], F32, "ExternalInput")
    b_s = k.dram("b_s", [2048], F32, "ExternalInput")
    w_out = k.dram("w_out", [2048, D], F32, "ExternalInput")
    io = declare_post_io(k, fused)
    hs = T(nc.dram_tensor("h_scr", [TOK, D], F32), "h_scr")
    with k.scope():
        ident = k.sb("ident", [128, 128])
        k.dma('sp', ident[:], io['idn'][:], writes=[ident])
        xT = k.sb("xT", [128, 8, TOK], BF16)
        w_inb = k.sb("w_inb", [128, 8, 4096], BF16)
        w_outb = k.sb("w_outb", [128, 16, D], BF16)
        WcT = k.sb("WcT", [128, 16, 128], BF16)
        ngb = k.sb("ngb", [128, 2048]); nbb = k.sb("nbb", [128, 2048]); bsb = k.sb("bsb", [128, 2048])
        pv = [k.ps("pv%d" % i, [128, 512]) for i in range(2)]
        pu = [k.ps("pu%d" % i, [128, 512]) for i in range(2)]
        pf = [k.ps("pf%d" % i, [128, 512]) for i in range(2)]
        ph = [k.ps("ph%d" % i, [128, 512]) for i in range(2)]
        k.dma('sp', ngb[:], ng[:].partition_broadcast(128), writes=[ngb])
        k.dma('act', nbb[:], nb[:].partition_broadcast(128), writes=[nbb])
        k.dma('sp', bsb[:], b_s[:].partition_broadcast(128), writes=[bsb])
        with k.scope():
            stg = Stager(k, 2, 4096)
            stg.load(w_inb, w_inb[:], w_in[:].rearrange("(kc p) n -> p kc n", p=128))
            stg.load(w_outb, w_outb[:], w_out[:].rearrange("(cc p) d -> p cc d", p=128))
            ws = k.sb("ws", [128, 16, 128])
            k.dma('sp', ws[:], w_s[:].rearrange("g t s -> t g s"), writes=[ws])
            k.op('pool', lambda e: e.affine_select(ws[:], ws[:], [[0, 16], [-1, 128]], ALU.is_ge, 0.0, base=0, channel_multiplier=1),
                 reads=[ws], writes=[ws])
            for g4 in range(4):
                pt = pv[g4 % 2]
                for gg in range(4):
                    g = g4 * 4 + gg
                    k.op('pe', lambda e: e.transpose(pt[:, gg * 128:(gg + 1) * 128], ws[:, g, :], ident[:]), reads=[ws, ident], writes=[pt])
                k.op('act', lambda e: e.copy(WcT[:, g4 * 4:g4 * 4 + 4, :], pt[:].rearrange("p (g t) -> p g t", g=4)), reads=[pt], writes=[WcT])
        load_xT(k, xT, x, ident, NT, pu + pf)
        v = k.sb("v", [128, 2048]); vb = k.sb("vb", [128, 2048], BF16)
        uT = k.sb("uT", [128, 16, 128], BF16); ufT = k.sb("ufT", [128, 16, 128], BF16)
        t1 = k.sb("t1", [128, 512])
        tmp = [k.sb("gt%d" % i, [128, 512]) for i in range(3)]
        st = k.sb("st", [128, 64])
        ht = [k.sb("ht%d" % i, [128, D]) for i in range(2)]
        for n in range(n_chunks):
            tok = slice(n * 128, (n + 1) * 128)
            for cb in range(4):
                p_ = pv[cb % 2]
                for kc in range(8):
                    k.mm(p_[:], xT[:, kc, tok], w_inb[:, kc, 2048 + cb * 512:2048 + (cb + 1) * 512], kc == 0, kc == 7,
                         reads=[xT, w_inb], writes=[p_])
                gelu_tanh(k, v[:, cb * 512:(cb + 1) * 512], v, p_, p_[:], tmp)
            ln_rows(k, v[:], v, 2048, ngb[:], nbb[:], ngb, nbb, st)
            k.op('pool', lambda e: e.tensor_copy(vb[:], v[:]), reads=[v], writes=[vb])
            for c4 in range(4):
                p_ = pu[c4 % 2]
                for cc in range(4):
                    c = c4 * 4 + cc
                    for kc in range(8):
                        k.mm(p_[:, cc * 128:(cc + 1) * 128], w_inb[:, kc, c * 128:(c + 1) * 128], xT[:, kc, tok], kc == 0, kc == 7,
                             reads=[xT, w_inb], writes=[p_])
                gelu_tanh(k, uT[:, c4 * 4:c4 * 4 + 4, :].rearrange("p c t -> p (c t)"), uT, p_, p_[:], tmp)
                q_ = pf[c4 % 2]
                for cc in range(4):
                    c = c4 * 4 + cc
                    k.mm(q_[:, cc * 128:(cc + 1) * 128], vb[:, c * 128:(c + 1) * 128], WcT[:, c, :], True, True,
                         reads=[vb, WcT], writes=[q_])
                k.op('dve', lambda e: e.tensor_tensor(t1[:].rearrange("p (c t) -> p c t", c=4), q_[:].rearrange("p (c t) -> p c t", c=4),
                                                      bsb[:, c4 * 512:(c4 + 1) * 512].rearrange("p (c t) -> p c t", c=4), ALU.add),
                     reads=[q_, bsb], writes=[t1])
                k.op('dve', lambda e: e.tensor_tensor(ufT[:, c4 * 4:c4 * 4 + 4, :].rearrange("p c t -> p (c t)"), t1[:],
                                                      uT[:, c4 * 4:c4 * 4 + 4, :].rearrange("p c t -> p (c t)"), ALU.mult),
                     reads=[t1, uT], writes=[ufT])
            h_ = ht[n % 2]
            for dh in range(2):
                for c in range(16):
                    k.mm(ph[dh][:], ufT[:, c, :], w_outb[:, c, dh * 512:(dh + 1) * 512], c == 0, c == 15, reads=[ufT, w_outb], writes=[ph[dh]])
                k.op('act', lambda e: e.copy(h_[:, dh * 512:(dh + 1) * 512], ph[dh][:]), reads=[ph[dh]], writes=[h_])
            k.dma('sp', hs[n * 128:(n + 1) * 128, :], h_[:], reads=[h_], writes=[hs])
    post(k, x, hs, io['lng'], io['lnb'], io['wr'], io['idn'], io['x1o'], io['x1T'], io['gto'])
    return tail(k, io)


A_GROUPS = ((128, 1), (512, 4), (2048, 16))
NEG = -30000.0
HQ = 4


def build_M_att(fused=True):
    nc, k = new_kb()
    x = k.dram("x", [TOK, D], F32, "ExternalInput")
    xh = k.dram("xh", [TOK, D], F32, "ExternalInput")
    w_in = k.dram("w_in", [D, 9216], F32, "ExternalInput")
    w_out = k.dram("w_out", [D, D], F32, "ExternalInput")
    tab = k.dram("tab", [3, 16, 128, 256], F32, "ExternalInput")
    negf = k.dram("negf", [128, 128], F32, "ExternalInput")
    io = declare_post_io(k, fused)
    hs = T(nc.dram_tensor("h_scr", [TOK, D], F32), "h_scr")
    Og = [T(nc.dram_tensor("O_scr%d" % g, [TOK, 16, 64], F32), "O_scr%d" % g) for g in range(3)]
    Lg = [T(nc.dram_tensor("L_scr%d" % g, [TOK, 16], F32), "L_scr%d" % g) for g in range(3)]
    with k.scope():
        ident = k.sb("ident", [128, 128])
        identb = k.sb("identb", [128, 128], BF16)
        k.dma('sp', ident[:], io['idn'][:], writes=[ident])
        k.op('dve', lambda e: e.tensor_copy(identb[:], ident[:]), reads=[ident], writes=[identb])
        xT = k.sb("xT", [128, 8, 2 * TOK], BF16)
        pq = [k.ps("pq%d" % i, [128, 512]) for i in range(2)]
        pS = [k.ps("pS%d" % i, [128, 512]) for i in range(2)]
        pP = [k.ps("pP%d" % i, [128, 256], BF16) for i in range(2)]
        pO = [k.ps("pO%d" % i, [128, 512]) for i in range(2)]
        load_xT(k, xT, xh, ident, NT, pq + pS, tok0=0)
        load_xT(k, xT, x, ident, NT, pq + pS, tok0=TOK)
        stg = Stager(k, 2, 2048)
        Wq = k.sb("Wq", [128, 8, HQ * 64], BF16); Wk = k.sb("Wk", [128, 8, HQ * 64], BF16); Wv = k.sb("Wv", [128, 8, HQ * 64], BF16)
        qT = k.sb("qT", [128, HQ // 2, TOK], BF16)
        kT = k.sb("kT", [128, HQ // 2, 2 * TOK], BF16)
        vv = k.sb("vv", [128, 32, HQ * 64], BF16)
        tb = k.sb("tb", [128, HQ, 256]); tb0 = k.sb("tb0", [128, HQ, 256])
        ngf = k.sb("ngf", [128, 128])
        k.dma('sp', ngf[:], negf[:], writes=[ngf])
        s_ = [k.sb("s%d" % i, [128, 256]) for i in range(2)]
        p_ = [k.sb("p%d" % i, [128, 256], BF16) for i in range(2)]
        PT = [k.sb("PT%d" % i, [128, 256], BF16) for i in range(2)]
        sm = [k.sb("sm%d" % i, [128, 8]) for i in range(2)]
        Ob = [k.sb("Ob%d" % i, [128, HQ, 64]) for i in range(2)]
        Lb = [k.sb("Lb%d" % i, [128, HQ]) for i in range(2)]
        cnt = dict(q=0, u=0, blk=0)
        for g, (window, d) in enumerate(A_GROUPS):
            n_own = TOK // d
            n_all = 2 * TOK // d
            nkb = n_all // 128
            nb = n_own // 128
            for hq in range(16 // HQ):
                c0 = hq * HQ * 64
                for j, W in enumerate((Wq, Wk, Wv)):
                    base = (g * 3 + j) * 1024 + c0
                    stg.load(W, W[:], w_in[:, base:base + HQ * 64].rearrange("(kc p) n -> p kc n", p=128))
                k.dma('sp', tb[:], tab[g, hq * HQ:(hq + 1) * HQ].rearrange("h q c -> q h c"), writes=[tb])
                k.op('pool', lambda e: e.tensor_copy(tb0[:, :, 128:256], tb[:, :, 128:256]), reads=[tb], writes=[tb0])
                k.op('pool', lambda e: e.tensor_tensor(tb0[:, :, 0:128], tb[:, :, 0:128], ngf[:].unsqueeze(1).to_broadcast([128, HQ, 128]), ALU.add),
                     reads=[tb, ngf], writes=[tb0])
                for r in range(d):
                    for (dst, W, lo, n) in ((qT, Wq, TOK + r, n_own), (kT, Wk, r, n_all)):
                        ch = min(512, n)
                        for c in range(n // ch):
                            for pp in range(HQ // 2):
                                pt = pq[cnt['q'] % 2]; cnt['q'] += 1
                                a = lo + d * c * ch
                                for kc in range(8):
                                    k.mm(pt[:, 0:ch], W[:, kc, pp * 128:(pp + 1) * 128], xT[:, kc, a:a + d * (ch - 1) + 1:d], kc == 0, kc == 7,
                                         reads=[W, xT], writes=[pt])
                                col = r * n + c * ch
                                k.op('act' if cnt['q'] % 2 else 'dve',
                                     lambda e: (e.copy if cnt['q'] % 2 else e.tensor_copy)(dst[:, pp, col:col + ch], pt[:, 0:ch]),
                                     reads=[pt], writes=[dst])
                    for kb in range(nkb):
                        pt = pq[cnt['q'] % 2]; cnt['q'] += 1
                        a = r + d * kb * 128
                        for kc in range(8):
                            k.mm(pt[:, 0:HQ * 64], xT[:, kc, a:a + d * 127 + 1:d], Wv[:, kc, :], kc == 0, kc == 7, reads=[Wv, xT], writes=[pt])
                        k.op('act' if cnt['q'] % 2 else 'dve',
                             lambda e: (e.copy if cnt['q'] % 2 else e.tensor_copy)(vv[:, r * nkb + kb, :], pt[:, 0:HQ * 64]),
                             reads=[pt], writes=[vv])
                for r in range(d):
                    for b in range(nb):
                        ob = Ob[cnt['blk'] % 2]; lb = Lb[cnt['blk'] % 2]; cnt['blk'] += 1
                        tbl = tb0 if b == 0 else tb
                        for h in range(HQ):
                            pp, hi = h // 2, h % 2
                            u = cnt['u'] % 2; cnt['u'] += 1
                            qa = qT[hi * 64:(hi + 1) * 64, pp, r * n_own + b * 128:r * n_own + (b + 1) * 128]
                            kc0 = r * n_all + n_own + 128 * (b - 1)
                            ka = kT[hi * 64:(hi + 1) * 64, pp, kc0:kc0 + 256]
                            k.mm(pS[u][:, 0:256], qa, ka, True, True, reads=[qT, kT], writes=[pS[u]])
                            k.op('dve', lambda e: e.scalar_tensor_tensor(s_[u][:], pS[u][:, 0:256], 0.125, tbl[:, h, :], ALU.mult, ALU.add),
                                 reads=[pS[u], tbl], writes=[s_[u]])
                            k.op('dve', lambda e: e.tensor_reduce(sm[u][:, 0:1], s_[u][:], AX.X, ALU.max), reads=[s_[u]], writes=[sm[u]])
                            k.op('dve', lambda e: e.tensor_scalar(sm[u][:, 1:2], sm[u][:, 0:1], -1.0, None, ALU.mult), reads=[sm[u]], writes=[sm[u]])
                            k.op('act', lambda e: e.activation(p_[u][:], s_[u][:], AF.Exp, bias=sm[u][:, 1:2], scale=1.0, accum_out=sm[u][:, 2:3]),
                                 reads=[s_[u], sm[u]], writes=[p_[u], sm[u]])
                            for half in range(2):
                                k.op('pe', lambda e: e.transpose(pP[u][:, half * 128:(half + 1) * 128], p_[u][:, half * 128:(half + 1) * 128], identb[:]),
                                     reads=[p_[u], identb], writes=[pP[u]])
                            k.op('act', lambda e: e.copy(PT[u][:], pP[u][:]), reads=[pP[u]], writes=[PT[u]])
                            kb0 = n_own // 128 + b - 1
                            for half in range(2):
                                k.mm(pO[u][:, 0:64], PT[u][:, half * 128:(half + 1) * 128], vv[:, r * nkb + kb0 + half, h * 64:(h + 1) * 64],
                                     half == 0, half == 1, reads=[PT[u], vv], writes=[pO[u]])
                            k.op('dve', lambda e: e.reciprocal(sm[u][:, 3:4], sm[u][:, 2:3]), reads=[sm[u]], writes=[sm[u]])
                            k.op('dve', lambda e: e.tensor_scalar(ob[:, h, :], pO[u][:, 0:64], sm[u][:, 3:4], None, ALU.mult),
                                 reads=[pO[u], sm[u]], writes=[ob])
                            k.op('act', lambda e: e.activation(sm[u][:, 4:5], sm[u][:, 2:3], AF.Ln), reads=[sm[u]], writes=[sm[u]])
                            k.op('dve', lambda e: e.tensor_tensor(lb[:, h:h + 1], sm[u][:, 4:5], sm[u][:, 0:1], ALU.add), reads=[sm[u]], writes=[lb])
                        row0 = r + d * 128 * b
                        rows = slice(row0, row0 + d * 127 + 1, d)
                        k.dma('sp', Og[g][rows, hq * HQ:(hq + 1) * HQ, :], ob[:], reads=[ob], writes=[Og[g]])
                        k.dma('act', Lg[g][rows, hq * HQ:(hq + 1) * HQ], lb[:], reads=[lb], writes=[Lg[g]])
    with k.scope():
        ident = k.sb("m_ident", [128, 128])
        k.dma('sp', ident[:], io['idn'][:], writes=[ident])
        w_outb = k.sb("m_wout", [128, 8, D], BF16)
        with k.scope():
            stg = Stager(k, 2, 4096)
            stg.load(w_outb, w_outb[:], w_out[:].rearrange("(kc p) d -> p kc d", p=128))
        O3 = [k.sb("m_O%d" % i, [128, 3, 16, 64]) for i in range(2)]
        L3 = [k.sb("m_L%d" % i, [128, 3, 16]) for i in range(2)]
        wk = [k.sb("m_w%d" % i, [128, 8, 16]) for i in range(2)]
        om = [k.sb("m_om%d" % i, [128, 16, 64]) for i in range(2)]
        ot = [k.sb("m_ot%d" % i, [128, 16, 64]) for i in range(2)]
        oT = [k.sb("m_oT%d" % i, [128, 8, 128], BF16) for i in range(2)]
        ht = [k.sb("m_h%d" % i, [128, D]) for i in range(2)]
        pT = [k.ps("m_pT%d" % i, [128, 512]) for i in range(2)]
        ph = [k.ps("m_ph%d" % i, [128, 512]) for i in range(2)]
        for j in range(NT):
            i = j % 2
            rows = slice(j * 128, (j + 1) * 128)
            for g in range(3):
                k.dma('sp', O3[i][:, g, :, :], Og[g][rows, :, :], reads=[Og[g]], writes=[O3[i]])
                k.dma('act', L3[i][:, g, :], Lg[g][rows, :], reads=[Lg[g]], writes=[L3[i]])
            w = wk[i]
            k.op('dve', lambda e: e.tensor_reduce(w[:, 0, :], L3[i][:].rearrange("p g h -> p h g"), AX.X, ALU.max), reads=[L3[i]], writes=[w])
            k.op('dve', lambda e: e.tensor_tensor(w[:, 1:4, :], L3[i][:], w[:, 0:1, :].to_broadcast([128, 3, 16]), ALU.subtract), reads=[L3[i], w], writes=[w])
            k.op('act', lambda e: e.activation(w[:, 1:4, :], w[:, 1:4, :], AF.Exp), reads=[w], writes=[w])
            k.op('dve', lambda e: e.tensor_reduce(w[:, 4, :], w[:, 1:4, :].rearrange("p g h -> p h g"), AX.X, ALU.add), reads=[w], writes=[w])
            k.op('dve', lambda e: e.reciprocal(w[:, 4, :], w[:, 4, :]), reads=[w], writes=[w])
            k.op('dve', lambda e: e.tensor_tensor(w[:, 5:8, :], w[:, 1:4, :], w[:, 4:5, :].to_broadcast([128, 3, 16]), ALU.mult), reads=[w], writes=[w])
            o_ = om[i]; t_ = ot[i]
            k.op('dve', lambda e: e.tensor_tensor(o_[:], O3[i][:, 0, :, :], w[:, 5, :].unsqueeze(2).to_broadcast([128, 16, 64]), ALU.mult), reads=[O3[i], w], writes=[o_])
            for g in (1, 2):
                k.op('pool', lambda e: e.tensor_tensor(t_[:], O3[i][:, g, :, :], w[:, 5 + g, :].unsqueeze(2).to_broadcast([128, 16, 64]), ALU.mult), reads=[O3[i], w], writes=[t_])
                k.op('dve', lambda e: e.tensor_tensor(o_[:], o_[:], t_[:], ALU.add), reads=[o_, t_], writes=[o_])
            of = o_[:].rearrange("p h c -> p (h c)")
            for hf in range(2):
                pt = pT[hf]
                for kk in range(4):
                    kc = hf * 4 + kk
                    k.op('pe', lambda e: e.transpose(pt[:, kk * 128:(kk + 1) * 128], of[:, kc * 128:(kc + 1) * 128], ident[:]), reads=[o_, ident], writes=[pt])
                k.op('act', lambda e: e.copy(oT[i][:, hf * 4:hf * 4 + 4, :], pt[:].rearrange("p (k t) -> p k t", k=4)), reads=[pt], writes=[oT[i]])
            h_ = ht[i]
            for dh in range(2):
                for kc in range(8):
                    k.mm(ph[dh][:], oT[i][:, kc, :], w_outb[:, kc, dh * 512:(dh + 1) * 512], kc == 0, kc == 7, reads=[oT[i], w_outb], writes=[ph[dh]])
                k.op('act', lambda e: e.copy(h_[:, dh * 512:(dh + 1) * 512], ph[dh][:]), reads=[ph[dh]], writes=[h_])
            k.dma('sp', hs[rows, :], h_[:], reads=[h_], writes=[hs])
    post(k, x, hs, io['lng'], io['lnb'], io['wr'], io['idn'], io['x1o'], io['x1T'], io['gto'])
    return tail(k, io)


def t5_bucket_np(dist):
    dist = np.asarray(dist)
    dd = np.maximum(dist, 1).astype(np.float32)
    large = 16 + (np.log(dd / np.float32(16)) / np.float32(math.log(2048 / 16)) * np.float32(16)).astype(np.int32)
    return np.where(dist < 16, dist, np.minimum(large, 31))


def att_tables(rel_bias):
    qi = np.arange(128)[:, None]
    ki = np.arange(256)[None, :]
    rel = qi + 128 - ki
    valid = (rel >= 0) & (rel <= 128)
    tabs = np.full((3, 16, 128, 256), NEG, np.float32)
    for g, (window, d) in enumerate(A_GROUPS):
        bk = t5_bucket_np(np.maximum(rel, 0) * d)
        b = rel_bias[bk]
        tabs[g] = np.where(valid[None], b.transpose(2, 0, 1), np.float32(NEG))
    return tabs


C_H = 8
CH = 64
SC = 128
RMS_EPS = 1e-6


def build_M_gdn(n_sc=2 * TOK // SC, fused=True):
    nc, k = new_kb()
    x = k.dram("x", [TOK, D], F32, "ExternalInput")
    xh = k.dram("xh", [TOK, D], F32, "ExternalInput")
    w_in = k.dram("w_in", [D, 4112], F32, "ExternalInput")
    conv = k.dram("conv", [4, 3072], F32, "ExternalInput")
    a_log = k.dram("a_log", [8], F32, "ExternalInput")
    dt_b = k.dram("dt_b", [8], F32, "ExternalInput")
    norm_w = k.dram("norm_w", [128], F32, "ExternalInput")
    w_out = k.dram("w_out", [D, D], F32, "ExternalInput")
    io = declare_post_io(k, fused)
    hs = T(nc.dram_tensor("h_scr", [TOK, D], F32), "h_scr")
    scale = 128 ** -0.5
    with k.scope():
        B = [k.ps("B%d" % i, [128, 512]) for i in range(8)]
        ident = k.sb("ident", [128, 128])
        k.dma('sp', ident[:], io['idn'][:], writes=[ident])
        ones = k.sb("ones", [128, 128])
        k.op('dve', lambda e: e.memset(ones[:], 1.0), writes=[ones])
        w_inb = k.sb("w_inb", [128, 8, 4112], BF16)
        w_outb = k.sb("w_outb", [128, 8, D], BF16)
        cwT = k.sb("cwT", [128, 4, 24])
        with k.scope():
            stg = Stager(k, 2, 4112)
            stg.load(w_inb, w_inb[:], w_in[:].rearrange("(kc p) n -> p kc n", p=128))
            stg.load(w_outb, w_outb[:], w_out[:].rearrange("(kc p) d -> p kc d", p=128))
            ct = k.sb("ct", [24, 4, 128])
            k.dma('sp', ct[:], conv[:].rearrange("j (fb p) -> fb j p", p=128), writes=[ct])
            for j in range(4):
                k.op('pe', lambda e: e.transpose(B[0][:, j * 24:(j + 1) * 24], ct[:, j, :], ident[0:24, 0:24]), reads=[ct, ident], writes=[B[0]])
            k.op('act', lambda e: e.copy(cwT[:].rearrange("p j f -> p (j f)"), B[0][:, 0:96]), reads=[B[0]], writes=[cwT])
        mLs = k.sb("mLs", [CH, 8, CH]); mUs = k.sb("mUs", [CH, 8, CH]); mUi = k.sb("mUi", [CH, 8, CH]); triU = k.sb("triU", [CH, CH])
        for (m, cmp_, pat, cm) in ((mLs, ALU.is_gt, [[0, 8], [-1, CH]], 1), (mUs, ALU.is_gt, [[0, 8], [1, CH]], -1),
                                   (mUi, ALU.is_ge, [[0, 8], [1, CH]], -1)):
            k.op('pool', lambda e: e.memset(m[:], 1.0), writes=[m])
            k.op('pool', lambda e: e.affine_select(m[:], m[:], pat, cmp_, 0.0, base=0, channel_multiplier=cm), reads=[m], writes=[m])
        k.op('pool', lambda e: e.memset(triU[:], 1.0), writes=[triU])
        k.op('pool', lambda e: e.affine_select(triU[:], triU[:], [[1, CH]], ALU.is_ge, 0.0, base=0, channel_multiplier=-1), reads=[triU], writes=[triU])
        cst = k.sb("cst", [CH, 32])
        k.dma('sp', cst[:, 0:8], dt_b[:].partition_broadcast(CH), writes=[cst])
        k.dma('sp', cst[:, 8:16], a_log[:].partition_broadcast(CH), writes=[cst])
        k.op('act', lambda e: e.activation(cst[:, 8:16], cst[:, 8:16], AF.Exp), reads=[cst], writes=[cst])
        k.op('dve', lambda e: e.tensor_scalar(cst[:, 8:16], cst[:, 8:16], -1.0, None, ALU.mult), reads=[cst], writes=[cst])
        nwb = k.sb("nwb", [CH, 128])
        k.dma('sp', nwb[:], norm_w[:].partition_broadcast(CH), writes=[nwb])
        xt = [k.sb("gx%d" % i, [128, D]) for i in range(2)]
        xTs = k.sb("xTs", [128, 8, SC], BF16)
        pre = k.sb("pre", [128, 24, SC + 3]); tl = k.sb("tl", [128, 24, 3])
        k.op('dve', lambda e: e.memset(pre[:], 0.0), writes=[pre])
        cv = k.sb("cv", [128, 24, SC]); cv2 = k.sb("cv2", [128, 24, SC])
        sq = k.sb("sq", [128, 16, CH]); rinv = k.sb("rinv", [128, 16, CH]); qkn = k.sb("qkn", [128, 16, CH])
        ktm = k.sb("ktm", [CH, 8, 128]); vtm = k.sb("vtm", [CH, 8, 128]); kd = k.sb("kd", [CH, 8, 128])
        sm = k.sb("sm", [CH, 16, 8])
        sm128 = k.sb("sm128", [128, 16])
        dg = k.sb("dg", [CH, 8, CH]); db = k.sb("db", [CH, 8, CH])
        E1 = k.sb("E1", [CH, 8, CH]); E2 = k.sb("E2", [CH, 8, CH]); E3 = k.sb("E3", [CH, 8, CH])
        A = k.sb("A", [CH, 8, CH]); AT = k.sb("AT", [CH, 8, CH]); IT = k.sb("IT", [CH, 8, CH])
        X = k.sb("X", [CH, 8, 256])
        kcT = k.sb("kcT", [128, 8, CH])
        S = k.sb("S", [128, 8, 128])
        k.op('dve', lambda e: e.memset(S[:], 0.0), writes=[S])
        vn = k.sb("vn", [CH, 128]); qs = k.sb("qs", [CH, 128])
        o = k.sb("o", [CH, 8, 128]); o2 = k.sb("o2", [CH, 8, 128]); zs = k.sb("zs", [CH, 8, 128])
        ogT = k.sb("ogT", [128, 8, CH], BF16)
        ht = k.sb("ht", [CH, D])
        beta, g_, G, GL, eG, kds, egs, bge, nbeta, ms = (sm[:, i, :] for i in range(10))

        def bch(a):
            return a.unsqueeze(2).to_broadcast([CH, 8, CH])
        for sc in range(n_sc):
            src = xh if sc * SC < TOK else x
            r0 = (sc * SC) % TOK
            x_ = xt[sc % 2]
            k.dma('sp' if sc % 2 == 0 else 'act', x_[:], src[r0:r0 + 128, :], writes=[x_])
            for hf in range(2):
                for kk in range(4):
                    kc = hf * 4 + kk
                    k.op('pe', lambda e: e.transpose(B[hf][:, kk * 128:(kk + 1) * 128], x_[:, kc * 128:(kc + 1) * 128], ident[:]),
                         reads=[x_, ident], writes=[B[hf]])
                k.op('act' if hf == 0 else 'dve',
                     lambda e: (e.copy if hf == 0 else e.tensor_copy)(xTs[:, hf * 4:hf * 4 + 4, :], B[hf][:].rearrange("p (k t) -> p k t", k=4)),
                     reads=[B[hf]], writes=[xTs])
            k.op('pool', lambda e: e.tensor_copy(tl[:], pre[:, :, SC:SC + 3]), reads=[pre], writes=[tl])
            for f4 in range(6):
                bk = B[2 + f4 % 2]
                for ff in range(4):
                    fb = f4 * 4 + ff
                    for kc in range(8):
                        k.mm(bk[:, ff * SC:(ff + 1) * SC], w_inb[:, kc, fb * 128:(fb + 1) * 128], xTs[:, kc, :], kc == 0, kc == 7,
                             reads=[w_inb, xTs], writes=[bk])
                k.op('act', lambda e: e.copy(pre[:, f4 * 4:f4 * 4 + 4, 3:SC + 3], bk[:].rearrange("p (f t) -> p f t", f=4)), reads=[bk, tl], writes=[pre])
            k.op('pool', lambda e: e.tensor_copy(pre[:, :, 0:3], tl[:]), reads=[tl], writes=[pre])
            for j in range(4):
                wj = cwT[:, j, :].unsqueeze(2).to_broadcast([128, 24, SC])
                if j == 0:
                    k.op('dve', lambda e: e.tensor_tensor(cv[:], pre[:, :, 0:SC], wj, ALU.mult), reads=[pre, cwT], writes=[cv])
                else:
                    k.op('pool', lambda e: e.tensor_tensor(cv2[:], pre[:, :, j:SC + j], wj, ALU.mult), reads=[pre, cwT], writes=[cv2])
                    k.op('dve', lambda e: e.tensor_tensor(cv[:], cv[:], cv2[:], ALU.add), reads=[cv, cv2], writes=[cv])
            k.op('act', lambda e: e.activation(cv[:], cv[:], AF.Silu), reads=[cv], writes=[cv])
            for cc in range(SC // CH):
                c_glob = sc * (SC // CH) + cc
                own = c_glob * CH >= TOK
                cols = slice(cc * CH, (cc + 1) * CH)
                for kc in range(8):
                    k.mm(B[4][0:CH, 0:16], xTs[:, kc, cols], w_inb[:, kc, 4096:4112], kc == 0, kc == 7, reads=[xTs, w_inb], writes=[B[4]])
                k.op('act', lambda e: e.activation(beta, B[4][0:CH, 0:8], AF.Sigmoid), reads=[B[4]], writes=[sm])
                k.op('dve', lambda e: e.tensor_tensor(g_, B[4][0:CH, 8:16], cst[:, 0:8], ALU.add), reads=[B[4], cst], writes=[sm])
                k.op('act', lambda e: e.activation(g_, g_, AF.Exp), reads=[sm], writes=[sm])
                k.op('dve', lambda e: e.tensor_scalar(g_, g_, 1.0, None, ALU.add), reads=[sm], writes=[sm])
                k.op('act', lambda e: e.activation(g_, g_, AF.Ln), reads=[sm], writes=[sm])
                k.op('dve', lambda e: e.tensor_tensor(g_, g_, cst[:, 8:16], ALU.mult), reads=[sm, cst], writes=[sm])
                k.mm(B[4][0:CH, 16:24], triU[:], g_, True, True, reads=[triU, sm], writes=[B[4]])
                k.mm(B[4][0:CH, 24:32], ones[0:CH, 0:CH], g_, True, True, reads=[ones, sm], writes=[B[4]])
                k.mm(B[4][:, 32:40], ones[0:CH, :], g_, True, True, reads=[ones, sm], writes=[B[4]])
                k.op('act', lambda e: e.copy(G, B[4][0:CH, 16:24]), reads=[B[4]], writes=[sm])
                k.op('act', lambda e: e.copy(GL, B[4][0:CH, 24:32]), reads=[B[4]], writes=[sm])
                k.op('act', lambda e: e.activation(sm128[:, 0:8], B[4][:, 32:40], AF.Exp), reads=[B[4]], writes=[sm128])
                k.op('act', lambda e: e.activation(eG, G, AF.Exp), reads=[sm], writes=[sm])
                k.op('dve', lambda e: e.tensor_tensor(kds, GL, G, ALU.subtract), reads=[sm], writes=[sm])
                k.op('act', lambda e: e.activation(kds, kds, AF.Exp), reads=[sm], writes=[sm])
                k.op('dve', lambda e: e.tensor_scalar(egs, eG, scale, None, ALU.mult), reads=[sm], writes=[sm])
                k.op('dve', lambda e: e.tensor_tensor(bge, beta, eG, ALU.mult), reads=[sm], writes=[sm])
                k.op('dve', lambda e: e.tensor_scalar(nbeta, beta, -1.0, None, ALU.mult), reads=[sm], writes=[sm])
                k.op('dve', lambda e: e.tensor_tensor(dg[:], ident[0:CH, 0:CH].unsqueeze(1).to_broadcast([CH, 8, CH]), bch(G), ALU.mult), reads=[ident, sm], writes=[dg])
                k.op('pool', lambda e: e.tensor_tensor(db[:], ident[0:CH, 0:CH].unsqueeze(1).to_broadcast([CH, 8, CH]), bch(beta), ALU.mult), reads=[ident, sm], writes=[db])
                k.mm(B[5][0:CH, :], ones[0:CH, 0:CH], dg[:].rearrange("p h f -> p (h f)"), True, True, reads=[ones, dg], writes=[B[5]])
                k.mm(B[6][0:CH, :], ones[0:CH, 0:CH], db[:].rearrange("p h f -> p (h f)"), True, True, reads=[ones, db], writes=[B[6]])
                Gb = B[5][0:CH, :].rearrange("p (h f) -> p h f", h=8)
                Bb = B[6][0:CH, :].rearrange("p (h f) -> p h f", h=8)
                k.op('dve', lambda e: e.scalar_tensor_tensor(E1[:], Gb, -1.0, bch(G), ALU.mult, ALU.add), reads=[B[5], sm], writes=[E1])
                k.op('dve', lambda e: e.tensor_scalar(E2[:], E1[:], -1.0, 0.0, ALU.mult, ALU.min), reads=[E1], writes=[E2])
                k.op('pool', lambda e: e.tensor_scalar(E1[:], E1[:], 0.0, None, ALU.min), reads=[E1], writes=[E1])
                k.op('act', lambda e: e.activation(E1[:], E1[:], AF.Exp), reads=[E1], writes=[E1])
                k.op('act', lambda e: e.activation(E2[:], E2[:], AF.Exp), reads=[E2], writes=[E2])
                k.op('dve', lambda e: e.scalar_tensor_tensor(E3[:], Bb, -1.0, E2[:], ALU.mult, ALU.mult), reads=[B[6], E2], writes=[E3])
                k.op('pool', lambda e: e.tensor_tensor(E3[:], E3[:], mUs[:], ALU.mult), reads=[E3, mUs], writes=[E3])
                k.op('dve', lambda e: e.tensor_tensor(E1[:], E1[:], bch(nbeta), ALU.mult), reads=[E1, sm], writes=[E1])
                k.op('pool', lambda e: e.tensor_tensor(E1[:], E1[:], mLs[:], ALU.mult), reads=[E1, mLs], writes=[E1])
                k.op('dve', lambda e: e.scalar_tensor_tensor(E2[:], E2[:], scale, mUi[:], ALU.mult, ALU.mult), reads=[E2, mUi], writes=[E2])
                k.op('dve', lambda e: e.tensor_tensor(sq[:], cv[:, 0:16, cols], cv[:, 0:16, cols], ALU.mult), reads=[cv], writes=[sq])
                for hf in range(2):
                    k.mm(B[hf][:], ones[:], sq[:, hf * 8:hf * 8 + 8, :].rearrange("p h t -> p (h t)"), True, True, reads=[ones, sq], writes=[B[hf]])
                    rv = rinv[:, hf * 8:hf * 8 + 8, :].rearrange("p h t -> p (h t)")
                    k.op('dve', lambda e: e.tensor_scalar(rv, B[hf][:], RMS_EPS, None, ALU.add), reads=[B[hf]], writes=[rinv])
                k.op('act', lambda e: e.activation(rinv[:], rinv[:], AF.Sqrt), reads=[rinv], writes=[rinv])
                k.op('dve', lambda e: e.reciprocal(rinv[:], rinv[:]), reads=[rinv], writes=[rinv])
                k.op('dve', lambda e: e.tensor_tensor(qkn[:], cv[:, 0:16, cols], rinv[:], ALU.mult), reads=[cv, rinv], writes=[qkn])
                for (dst, srcf) in ((ktm, lambda h: qkn[:, 8 + h, :]), (vtm, lambda h: cv[:, 16 + h, cols])):
                    for hf in range(2):
                        for hh in range(4):
                            h = hf * 4 + hh
                            k.op('pe', lambda e: e.transpose(B[hf][0:CH, hh * 128:(hh + 1) * 128], srcf(h), ident[:]), reads=[qkn, cv, ident], writes=[B[hf]])
                        k.op('act' if hf == 0 else 'dve',
                             lambda e: (e.copy if hf == 0 else e.tensor_copy)(dst[:, hf * 4:hf * 4 + 4, :], B[hf][0:CH, :].rearrange("p (h d) -> p h d", h=4)),
                             reads=[B[hf]], writes=[dst])
                for h in range(8):
                    k.mm(B[2][0:CH, h * CH:(h + 1) * CH], qkn[:, 8 + h, :], qkn[:, 8 + h, :], True, True, reads=[qkn], writes=[B[2]])
                    k.mm(B[3][0:CH, h * CH:(h + 1) * CH], qkn[:, 8 + h, :], qkn[:, h, :], True, True, reads=[qkn], writes=[B[3]])
                P1 = B[2][0:CH, :].rearrange("p (h f) -> p h f", h=8)
                k.op('dve', lambda e: e.tensor_tensor(A[:], P1, E1[:], ALU.mult), reads=[B[2], E1], writes=[A])
                k.op('dve', lambda e: e.tensor_tensor(AT[:], P1, E3[:], ALU.mult), reads=[B[2], E3], writes=[AT])
                k.op('dve', lambda e: e.tensor_tensor(IT[:], B[3][0:CH, :].rearrange("p (h f) -> p h f", h=8), E2[:], ALU.mult), reads=[B[3], E2], writes=[IT])
                k.op('dve', lambda e: e.tensor_tensor(X[:, :, 0:128], vtm[:], beta.unsqueeze(2).to_broadcast([CH, 8, 128]), ALU.mult), reads=[vtm, sm], writes=[X])
                k.op('pool', lambda e: e.tensor_tensor(X[:, :, 128:256], ktm[:], bge.unsqueeze(2).to_broadcast([CH, 8, 128]), ALU.mult), reads=[ktm, sm], writes=[X])
                k.op('pool', lambda e: e.tensor_tensor(kd[:], ktm[:], kds.unsqueeze(2).to_broadcast([CH, 8, 128]), ALU.mult), reads=[ktm, sm], writes=[kd])
                for lv in range(6):
                    for h in range(8):
                        k.mm(B[4 + h // 2][0:CH, (h % 2) * 256:(h % 2 + 1) * 256], AT[:, h, :], X[:, h, :], True, True, reads=[AT, X], writes=[B[4 + h // 2]])
                    if lv < 5:
                        for h in range(8):
                            k.mm(B[2][0:CH, h * CH:(h + 1) * CH], AT[:, h, :], A[:, h, :], True, True, reads=[AT, A], writes=[B[2]])
                            k.mm(B[3][0:CH, h * CH:(h + 1) * CH], A[:, h, :], AT[:, h, :], True, True, reads=[AT, A], writes=[B[3]])
                    for q4 in range(4):
                        xv = X[:, q4 * 2:q4 * 2 + 2, :].rearrange("p h d -> p (h d)")
                        k.op('dve', lambda e: e.tensor_tensor(xv, xv, B[4 + q4][0:CH, :], ALU.add), reads=[X, B[4 + q4]], writes=[X])
                    if lv < 5:
                        k.op('act', lambda e: e.copy(A[:].rearrange("p h f -> p (h f)"), B[2][0:CH, :]), reads=[B[2]], writes=[A])
                        k.op('act', lambda e: e.copy(AT[:].rearrange("p h f -> p (h f)"), B[3][0:CH, :]), reads=[B[3]], writes=[AT])
                for h in range(8):
                    k.op('pe', lambda e: e.transpose(B[0][:, h * CH:(h + 1) * CH], X[:, h, 128:256], ident[0:CH, 0:CH]), reads=[X, ident], writes=[B[0]])
                k.op('act', lambda e: e.copy(kcT[:].rearrange("p h c -> p (h c)"), B[0][:]), reads=[B[0]], writes=[kcT])
                if own:
                    for hf in range(2):
                        for kc in range(8):
                            k.mm(B[6 + hf][0:CH, :], xTs[:, kc, cols], w_inb[:, kc, 3072 + hf * 512:3072 + (hf + 1) * 512], kc == 0, kc == 7,
                                 reads=[xTs, w_inb], writes=[B[6 + hf]])
                        k.op('act', lambda e: e.activation(zs[:, hf * 4:hf * 4 + 4, :].rearrange("p h d -> p (h d)"), B[6 + hf][0:CH, :], AF.Silu),
                             reads=[B[6 + hf]], writes=[zs])
                for h in range(8):
                    k.mm(B[1][0:CH, 0:128], kcT[:, h, :], S[:, h, :], True, True, reads=[kcT, S], writes=[B[1]])
                    k.op('dve', lambda e: e.tensor_tensor(vn[:], X[:, h, 0:128], B[1][0:CH, 0:128], ALU.subtract), reads=[X, B[1]], writes=[vn])
                    if own:
                        k.mm(B[2][0:CH, 0:128], qkn[:, h, :], S[:, h, :], True, True, reads=[qkn, S], writes=[B[2]])
                        k.mm(B[3][0:CH, 0:128], IT[:, h, :], vn[:], True, True, reads=[IT, vn], writes=[B[3]])
                        k.op('act', lambda e: e.activation(qs[:], B[2][0:CH, 0:128], AF.Copy, scale=egs[:, h:h + 1]), reads=[B[2], sm], writes=[qs])
                        k.op('dve', lambda e: e.tensor_tensor(o[:, h, :], qs[:], B[3][0:CH, 0:128], ALU.add), reads=[qs, B[3]], writes=[o])
                    k.mm(B[0][:, 0:128], kd[:, h, :], vn[:], True, True, reads=[kd, vn], writes=[B[0]])
                    k.op('dve', lambda e: e.scalar_tensor_tensor(S[:, h, :], S[:, h, :], sm128[:, h:h + 1], B[0][:, 0:128], ALU.mult, ALU.add),
                         reads=[S, sm128, B[0]], writes=[S])
                if own:
                    k.op('pool', lambda e: e.tensor_tensor(o2[:], o[:], o[:], ALU.mult), reads=[o], writes=[o2])
                    k.op('dve', lambda e: e.tensor_reduce(ms, o2[:], AX.X, ALU.add), reads=[o2], writes=[sm])
                    k.op('dve', lambda e: e.tensor_scalar(ms, ms, 1.0 / 128, RMS_EPS, ALU.mult, ALU.add), reads=[sm], writes=[sm])
                    k.op('act', lambda e: e.activation(ms, ms, AF.Sqrt), reads=[sm], writes=[sm])
                    k.op('dve', lambda e: e.reciprocal(ms, ms), reads=[sm], writes=[sm])
                    k.op('dve', lambda e: e.tensor_tensor(o[:], o[:], ms.unsqueeze(2).to_broadcast([CH, 8, 128]), ALU.mult), reads=[o, sm], writes=[o])
                    k.op('pool', lambda e: e.tensor_tensor(o[:], o[:], nwb[:].unsqueeze(1).to_broadcast([CH, 8, 128]), ALU.mult), reads=[o, nwb], writes=[o])
                    k.op('dve', lambda e: e.tensor_tensor(o[:], o[:], zs[:], ALU.mult), reads=[o, zs], writes=[o])
                    for h in range(8):
                        k.op('pe', lambda e: e.transpose(B[4][:, h * CH:(h + 1) * CH], o[:, h, :], ident[0:CH, 0:CH]), reads=[o, ident], writes=[B[4]])
                    k.op('act', lambda e: e.copy(ogT[:].rearrange("p h c -> p (h c)"), B[4][:]), reads=[B[4]], writes=[ogT])
                    for dh in range(2):
                        for kc in range(8):
                            k.mm(B[5 + dh][0:CH, :], ogT[:, kc, :], w_outb[:, kc, dh * 512:(dh + 1) * 512], kc == 0, kc == 7,
                                 reads=[ogT, w_outb], writes=[B[5 + dh]])
                        k.op('act', lambda e: e.copy(ht[:, dh * 512:(dh + 1) * 512], B[5 + dh][0:CH, :]), reads=[B[5 + dh]], writes=[ht])
                    t0 = c_glob * CH - TOK
                    k.dma('sp', hs[t0:t0 + CH, :], ht[:], reads=[ht], writes=[hs])
    post(k, x, hs, io['lng'], io['lnb'], io['wr'], io['idn'], io['x1o'], io['x1T'], io['gto'])
    return tail(k, io)


_BUILD_CACHE = {}


def _get(name, fn):
    if name not in _BUILD_CACHE:
        _BUILD_CACHE[name] = fn()
    return _BUILD_CACHE[name]


def _run(nc, in_maps):
    res = run_bass_kernel_spmd(nc, in_maps, core_ids=list(range(NCORES)))
    return res.results


def kernel(x, rel_bias, a_w_in, a_w_out, b_w_in, b_norm_g, b_norm_b, b_w_s, b_b_s, b_w_out,
           c_w_in, c_conv, c_a_log, c_dt_bias, c_norm_w, c_w_out, ln_g, ln_b,
           moe_w_coarse, moe_w_fine, moe_w_gate, moe_w_up, moe_w_down):
    f32 = lambda a: np.ascontiguousarray(np.asarray(a), dtype=np.float32)
    x = f32(x)
    Bn, S, Dm = x.shape
    xs = x.reshape(Bn * S, Dm)
    cur = [xs[c * TOK:(c + 1) * TOK] for c in range(NCORES)]
    idn = np.eye(128, dtype=np.float32)
    zeros = np.zeros((TOK, D), np.float32)
    tabs = att_tables(f32(rel_bias))
    negf = [np.full((128, 128), NEG, np.float32), np.zeros((128, 128), np.float32)]
    depth = ln_g.shape[0]
    for i in range(depth):
        kind, j = i % 3, i // 3
        wr = np.ascontiguousarray(np.concatenate([f32(moe_w_coarse[i]), f32(moe_w_fine[i]).reshape(D, 64)], axis=1))
        pio = dict(lng=f32(ln_g[i, 0]), lnb=f32(ln_b[i, 0]), wr=wr, idn=idn,
                   wg=f32(moe_w_gate[i]), wu=f32(moe_w_up[i]), wd=f32(moe_w_down[i]),
                   lng2=f32(ln_g[i, 1]), lnb2=f32(ln_b[i, 1]))
        halo = [cur[c - 1] if c % 2 == 1 else zeros for c in range(NCORES)]
        if kind == 0:
            nc = build_M_att()
            w_in, w_out = f32(a_w_in[j]), f32(a_w_out[j])
            maps = [dict(x=cur[c], xh=halo[c], w_in=w_in, w_out=w_out, tab=tabs, negf=negf[c % 2], **pio) for c in range(NCORES)]
        elif kind == 1:
            nc = build_M_sgu()
            ws = dict(w_in=f32(b_w_in[j]), ng=f32(b_norm_g[j]), nb=f32(b_norm_b[j]), w_s=f32(b_w_s[j]),
                      b_s=f32(b_b_s[j]).reshape(2048), w_out=f32(b_w_out[j]))
            maps = [dict(x=cur[c], **ws, **pio) for c in range(NCORES)]
        else:
            nc = build_M_gdn()
            ws = dict(w_in=f32(c_w_in[j]), conv=f32(c_conv[j]), a_log=f32(c_a_log[j]), dt_b=f32(c_dt_bias[j]),
                      norm_w=f32(c_norm_w[j]), w_out=f32(c_w_out[j]))
            maps = [dict(x=cur[c], xh=halo[c], **ws, **pio) for c in range(NCORES)]
        res = _run(nc, maps)
        cur = [np.asarray(r['x2']) for r in res]
    return np.concatenate(cur, axis=0).reshape(Bn, S, Dm).astype(np.float32)
```

```python
import numpy as np
import ml_dtypes
import concourse.bass as bass
import concourse.mybir as mybir
from concourse.bass_utils import run_bass_kernel_spmd

F32 = mybir.dt.float32
BF16 = mybir.dt.bfloat16
I32 = mybir.dt.int32
U32 = mybir.dt.uint32
AF = mybir.ActivationFunctionType
ALU = mybir.AluOpType
AX = mybir.AxisListType

N_DMA_SEMS = 24


class T:
    def __init__(self, h, name):
        self.h = h
        self.name = name
        self.excl = 'psum' in str(getattr(h, 'space', '')).lower() or 'psum' in str(type(h)).lower()
        self.w = None
        self.r = {}

    def __getitem__(self, idx):
        return self.h[idx]


class KB:
    def __init__(self, nc, same_engine_sync=True):
        self.nc = nc
        self.eng = dict(pe=nc.tensor, dve=nc.vector, act=nc.scalar, pool=nc.gpsimd, sp=nc.sync)
        self.sem = {e: nc.alloc_semaphore("s_" + e) for e in self.eng}
        self.cnt = {e: 0 for e in self.eng}
        self.waited = {e: {} for e in self.eng}
        self.dsem = [nc.alloc_semaphore("s_dma%d" % i) for i in range(N_DMA_SEMS)]
        self.dcnt = [0] * N_DMA_SEMS
        self.drr = 0
        self.same_engine_sync = same_engine_sync
        self.n_inst = 0
        self.n_wait = 0
        self.out_marks = []

    def sb(self, name, shape, dtype=F32):
        return T(self.nc.alloc_sbuf_tensor(name, list(shape), dtype), name)

    def ps(self, name, shape, dtype=F32):
        return T(self.nc.alloc_psum_tensor(name, list(shape), dtype), name)

    def dram(self, name, shape, dtype, kind):
        return T(self.nc.dram_tensor(name, list(shape), dtype, kind=kind), name)

    def _semh(self, key):
        return self.sem[key] if isinstance(key, str) else self.dsem[key]

    def _wait(self, e, key, val):
        if key == e and (e == 'pe' or not self.same_engine_sync):
            return
        if self.waited[e].get(key, 0) >= val:
            return
        self.eng[e].wait_ge(self._semh(key), val)
        self.waited[e][key] = val
        self.n_wait += 1

    def _deps(self, e, reads, writes):
        deps = {}

        def add(m):
            if m is None:
                return
            k, v = m
            if deps.get(k, 0) < v:
                deps[k] = v
        for t in reads:
            add(t.w)
            if t.excl:
                for k, v in t.r.items():
                    if k != e:
                        add((k, v))
        for t in writes:
            add(t.w)
            for k, v in t.r.items():
                add((k, v))
        for k, v in deps.items():
            self._wait(e, k, v)

    def _mark(self, mark, reads, writes):
        k, v = mark
        for t in reads:
            if t.r.get(k, 0) < v:
                t.r[k] = v
        for t in writes:
            t.w = mark
            t.r = {}

    def op(self, e, fn, reads=(), writes=(), ser=False):
        self._deps(e, reads, writes)
        if ser and self.cnt[e] and self.waited[e].get(e, 0) < self.cnt[e]:
            self.eng[e].wait_ge(self.sem[e], self.cnt[e])
            self.waited[e][e] = self.cnt[e]
        inst = fn(self.eng[e])
        self.cnt[e] += 1
        inst.then_inc(self.sem[e], 1)
        self.n_inst += 1
        self._mark((e, self.cnt[e]), reads, writes)
        return inst

    def mm(self, out_ap, lhsT, rhs, start, stop, reads=(), writes=(), **kw):
        return self.op('pe', lambda en: en.matmul(out_ap, lhsT, rhs, start=start, stop=stop, **kw),
                       reads=reads, writes=writes)

    def dma(self, q, out_ap, in_ap, reads=(), writes=(), **kw):
        self._deps(q, reads, writes)
        i = self.drr
        self.drr = (self.drr + 1) % N_DMA_SEMS
        self._wait(q, i, self.dcnt[i])
        inst = self.eng[q].dma_start(out=out_ap, in_=in_ap, **kw)
        self.dcnt[i] += 16
        inst.then_inc(self.dsem[i], 16)
        self.n_inst += 1
        self._mark((i, self.dcnt[i]), reads, writes)
        return (i, self.dcnt[i])

    def barrier(self):
        for e in ('pe', 'dve', 'act', 'pool', 'sp'):
            for i in range(N_DMA_SEMS):
                if self.dcnt[i]:
                    self._wait(e, i, self.dcnt[i])
            for o in ('pe', 'dve', 'act', 'pool'):
                if o != e and self.cnt[o]:
                    self._wait(e, o, self.cnt[o])

    def finish(self, out_tiles):
        for i in range(N_DMA_SEMS):
            self._wait('sp', i, self.dcnt[i])
        for e in ('pe', 'dve', 'act', 'pool'):
            if self.cnt[e]:
                self._wait('sp', e, self.cnt[e])

import contextlib
import math

TOK = 2048
D = 1024
NT = TOK // 128
ALPHA = (2 * 4) ** 0.25
LN_EPS = 1e-5
NCORES = 8


def _scope_patch():
    def scope(self):
        @contextlib.contextmanager
        def cm():
            st = contextlib.ExitStack()
            self._stacks.append(st)
            try:
                with st:
                    yield
                    self.barrier()
            finally:
                self._stacks.pop()
        return cm()

    def sb(self, name, shape, dtype=F32):
        self._uid = getattr(self, '_uid', 0) + 1
        name = "%s_%d" % (name, self._uid)
        if getattr(self, '_stacks', None):
            h = self._stacks[-1].enter_context(self.nc.sbuf_tensor(name, list(shape), dtype))
        else:
            h = self.nc.alloc_sbuf_tensor(name, list(shape), dtype)
        return T(h, name)

    def ps(self, name, shape, dtype=F32):
        self._uid = getattr(self, '_uid', 0) + 1
        name = "%s_%d" % (name, self._uid)
        if getattr(self, '_stacks', None):
            h = self._stacks[-1].enter_context(self.nc.psum_tensor(name, list(shape), dtype))
        else:
            h = self.nc.alloc_psum_tensor(name, list(shape), dtype)
        return T(h, name)
    KB.scope = scope
    KB.sb = sb
    KB.ps = ps


_scope_patch()


def new_kb():
    nc = bass.Bass("TRN2", target_bir_lowering=False)
    k = KB(nc, same_engine_sync=True)
    k._stacks = []
    return nc, k


class Stager:
    def __init__(self, k, n=2, width=4096):
        self.k = k
        self.width = width
        self.bufs = [k.sb("stage%d" % i, [128, width]) for i in range(n)]
        self.i = 0

    def load(self, dst_t, dst_ap, src_ap, eng='pool'):
        k = self.k
        shp = list(dst_ap.shape)
        if len(shp) == 2:
            dst_ap = dst_ap.unsqueeze(1)
            src_ap = src_ap.unsqueeze(1)
            shp = list(dst_ap.shape)
        p, a, b = shp
        assert b <= self.width
        step = max(1, self.width // b)
        for a0 in range(0, a, step):
            a1 = min(a, a0 + step)
            st = self.bufs[self.i % len(self.bufs)]
            q = 'sp' if self.i % 2 == 0 else 'act'
            self.i += 1
            sv = st[0:p, 0:(a1 - a0) * b].rearrange("p (a b) -> p a b", b=b)
            k.dma(q, sv, src_ap[:, a0:a1, :], writes=[st])
            k.op(eng, lambda en: en.tensor_copy(dst_ap[:, a0:a1, :], sv), reads=[st], writes=[dst_t])


def ln_rows(k, ya, yt, W, gb, bb, gbt, bbt, st, eng2='pool', eps=LN_EPS):
    nch = W // 512
    for c in range(nch):
        k.op('dve', lambda e: e.bn_stats(st[:, 6 * c:6 * c + 6], ya[:, c * 512:(c + 1) * 512]), reads=[yt], writes=[st])
    mv = st[:, 48:50]
    rs = st[:, 50:51]
    k.op('dve', lambda e: e.bn_aggr(mv, st[:, 0:6 * nch]), reads=[st], writes=[st])
    k.op('dve', lambda e: e.tensor_scalar(rs, st[:, 49:50], eps, None, ALU.add), reads=[st], writes=[st])
    k.op('act', lambda e: e.activation(rs, rs, AF.Sqrt), reads=[st], writes=[st])
    k.op('dve', lambda e: e.reciprocal(rs, rs), reads=[st], writes=[st])
    k.op('dve', lambda e: e.tensor_scalar(ya, ya, st[:, 48:49], rs, ALU.subtract, ALU.mult), reads=[yt, st], writes=[yt])
    k.op(eng2, lambda e: e.tensor_tensor(ya, ya, gb, ALU.mult), reads=[yt, gbt], writes=[yt])
    k.op(eng2, lambda e: e.tensor_tensor(ya, ya, bb, ALU.add), reads=[yt, bbt], writes=[yt])


def post(k, x_src, h_src, lng, lnb, wr, idn, x1_out, x1T_out, gt_out):
    with k.scope():
        ident = k.sb("p_ident", [128, 128])
        wrt = k.sb("p_wrt", [128, 8, 72])
        gb = k.sb("p_gb", [128, D]); bb = k.sb("p_bb", [128, D])
        xt = [k.sb("p_x%d" % i, [128, D]) for i in range(2)]
        ht = [k.sb("p_h%d" % i, [128, D]) for i in range(2)]
        xT32 = [k.sb("p_xT32_%d" % i, [128, 8, 128]) for i in range(2)]
        xTb = [k.sb("p_xTb%d" % i, [128, 8, 128], BF16) for i in range(2)]
        st = [k.sb("p_st%d" % i, [128, 64]) for i in range(2)]
        L = k.sb("p_L", [128, NT, 72])
        GT = k.sb("p_GT", [64, TOK])
        R = k.sb("p_R", [128, 4096])
        pT = [k.ps("p_pT%d" % i, [128, 512]) for i in range(4)]
        pL = [k.ps("p_pL%d" % i, [128, 512]) for i in range(2)]
        k.dma('sp', ident[:], idn[:], writes=[ident])
        k.dma('sp', wrt[:], wr[:].rearrange("(kc p) n -> p kc n", p=128), writes=[wrt])
        k.dma('sp', gb[:], lng[:].partition_broadcast(128), writes=[gb])
        k.dma('act', bb[:], lnb[:].partition_broadcast(128), writes=[bb])
        for j in range(NT):
            x_ = xt[j % 2]; h_ = ht[j % 2]
            k.dma('sp', x_[:], x_src[j * 128:(j + 1) * 128, :], writes=[x_])
            k.dma('act', h_[:], h_src[j * 128:(j + 1) * 128, :], reads=[h_src] if isinstance(h_src, T) else [], writes=[h_])
            k.op('dve', lambda e: e.scalar_tensor_tensor(x_[:], x_[:], ALPHA, h_[:], ALU.mult, ALU.add), reads=[x_, h_], writes=[x_])
            ln_rows(k, x_[:], x_, D, gb[:], bb[:], gb, bb, st[j % 2])
            k.dma('sp', x1_out[j * 128:(j + 1) * 128, :], x_[:], reads=[x_], writes=[])
            x32 = xT32[j % 2]; xb = xTb[j % 2]
            for hf in range(2):
                pt = pT[(2 * j + hf) % 4]
                for kk in range(4):
                    kc = hf * 4 + kk
                    k.op('pe', lambda e: e.transpose(pt[:, kk * 128:(kk + 1) * 128], x_[:, kc * 128:(kc + 1) * 128], ident[:]),
                         reads=[x_, ident], writes=[pt])
                k.op('act', lambda e: e.copy(x32[:, hf * 4:hf * 4 + 4, :], pt[:].rearrange("p (k t) -> p k t", k=4)), reads=[pt], writes=[x32])
            k.op('pool', lambda e: e.tensor_copy(xb[:], x32[:]), reads=[x32], writes=[xb])
            k.dma('act', x1T_out[:, j * 128:(j + 1) * 128].rearrange("(kc p) t -> p kc t", p=128), xb[:], reads=[xb], writes=[])
            pl = pL[j % 2]
            for kc in range(8):
                k.mm(pl[:, 0:72], x32[:, kc, :], wrt[:, kc, :], kc == 0, kc == 7, reads=[x32, wrt], writes=[pl])
            k.op('act', lambda e: e.copy(L[:, j, :], pl[:, 0:72]), reads=[pl], writes=[L])
        off = [0]

        def rt(n):
            a = R[:, off[0]:off[0] + n]
            off[0] += n
            return a

        def v3(a):
            return a.rearrange("p (j g) -> p j g", j=NT)
        cm = rt(NT); cs = v3(rt(NT * 8)); ohg = v3(rt(NT * 8)); ce = v3(rt(NT * 8)); csum = rt(NT); pgrp = rt(NT)
        fm = rt(NT * 64).rearrange("p (j g e) -> p j g e", j=NT, g=8)
        fsel = v3(rt(NT * 8)); m2 = rt(NT); fs = v3(rt(NT * 8)); mk1 = v3(rt(NT * 8)); fs2 = v3(rt(NT * 8))
        m3 = rt(NT); mk2 = v3(rt(NT * 8)); eb = rt(NT); den = rt(NT); g1 = rt(NT); g2 = rt(NT)
        t1 = v3(rt(NT * 8)); t2 = v3(rt(NT * 8)); Gf = v3(rt(NT * 8))
        G = rt(NT * 64).rearrange("p (j g e) -> p j g e", j=NT, g=8)

        def bc3(a):
            return a.unsqueeze(2).to_broadcast([128, NT, 8])

        def r(eng, fn):
            k.op(eng, fn, reads=[R, L], writes=[R])
        Lc = L[:, :, 0:8]
        Lf = L[:, :, 8:72].rearrange("p j (g e) -> p j g e", g=8)
        ohg4 = ohg.unsqueeze(3).to_broadcast([128, NT, 8, 8])
        r('dve', lambda e: e.tensor_reduce(cm, Lc, AX.X, ALU.max))
        r('dve', lambda e: e.tensor_tensor(cs, Lc, bc3(cm), ALU.subtract))
        r('dve', lambda e: e.tensor_single_scalar(ohg, cs, 0.0, ALU.is_equal))
        r('act', lambda e: e.activation(ce, cs, AF.Exp))
        r('dve', lambda e: e.tensor_reduce(csum, ce, AX.X, ALU.add))
        r('dve', lambda e: e.reciprocal(pgrp, csum))
        r('dve', lambda e: e.tensor_tensor(fm, Lf, ohg4, ALU.mult))
        r('dve', lambda e: e.tensor_reduce(fsel, fm.rearrange("p j g e -> p j e g"), AX.X, ALU.add))
        r('dve', lambda e: e.tensor_reduce(m2, fsel, AX.X, ALU.max))
        r('dve', lambda e: e.tensor_tensor(fs, fsel, bc3(m2), ALU.subtract))
        r('dve', lambda e: e.tensor_single_scalar(mk1, fs, 0.0, ALU.is_equal))
        r('dve', lambda e: e.scalar_tensor_tensor(fs2, mk1, -1e30, fs, ALU.mult, ALU.add))
        r('dve', lambda e: e.tensor_reduce(m3, fs2, AX.X, ALU.max))
        r('dve', lambda e: e.tensor_tensor(mk2, fs2, bc3(m3), ALU.is_equal))
        r('act', lambda e: e.activation(eb, m3, AF.Exp))
        r('dve', lambda e: e.tensor_scalar(den, eb, 1.0, None, ALU.add))
        r('dve', lambda e: e.reciprocal(den, den))
        r('dve', lambda e: e.tensor_tensor(g1, pgrp, den, ALU.mult))
        r('dve', lambda e: e.tensor_tensor(g2, g1, eb, ALU.mult))
        r('dve', lambda e: e.tensor_tensor(t1, mk1, bc3(g1), ALU.mult))
        r('dve', lambda e: e.tensor_tensor(t2, mk2, bc3(g2), ALU.mult))
        r('dve', lambda e: e.tensor_tensor(Gf, t1, t2, ALU.add))
        r('dve', lambda e: e.tensor_tensor(G, ohg4, Gf.unsqueeze(2).to_broadcast([128, NT, 8, 8]), ALU.mult))
        for j4 in range(NT // 4):
            pt = pT[j4 % 4]
            for jj in range(4):
                j = j4 * 4 + jj
                k.op('pe', lambda e: e.transpose(pt[0:64, jj * 128:(jj + 1) * 128],
                                                  G[:, j, :, :].rearrange("p g e -> p (g e)"), ident[:]),
                     reads=[R, ident], writes=[pt])
            k.op('act', lambda e: e.copy(GT[:, j4 * 512:(j4 + 1) * 512], pt[0:64, 0:512]), reads=[pt], writes=[GT])
        k.dma('sp', gt_out[:, :], GT[:], reads=[GT], writes=[])


NTOKALL = 16384
HID = 512
EPC = 8


def build_E(n_blocks=NTOKALL // TOK, n_exp=EPC):
    nc, k = new_kb()
    ntok = n_blocks * TOK
    xT = k.dram("xT", [D, ntok], BF16, "ExternalInput")
    gt = k.dram("gt", [EPC, ntok], F32, "ExternalInput")
    wg = k.dram("wg", [EPC, D, HID], F32, "ExternalInput")
    wu = k.dram("wu", [EPC, D, HID], F32, "ExternalInput")
    wd = k.dram("wd", [EPC, HID, D], F32, "ExternalInput")
    idn = k.dram("idn", [128, 128], F32, "ExternalInput")
    yp = k.dram("yp", [ntok, D], BF16, "ExternalOutput")
    ident = k.sb("ident", [128, 128])
    k.dma('sp', ident[:], idn[:], writes=[ident])
    yacc_h = nc.alloc_sbuf_tensor("yacc", [128, NT, D], F32)
    yacc = [[T(yacc_h, "yacc%d_%d" % (j, h)) for h in range(2)] for j in range(NT)]
    xTb = k.sb("xTb", [128, 8, TOK], BF16)
    GTb = k.sb("GTb", [EPC, TOK])
    Wg = [k.sb("Wg%d" % i, [128, 8, HID], BF16) for i in range(2)]
    Wu = [k.sb("Wu%d" % i, [128, 8, HID], BF16) for i in range(2)]
    Wd = [k.sb("Wd%d" % i, [128, 4, D], BF16) for i in range(2)]
    stg = Stager(k, 2, 4096)
    hT = [k.sb("hT%d" % i, [128, 4, 512], BF16) for i in range(2)]
    sg = [k.sb("sg%d" % i, [128, 512], BF16) for i in range(2)]
    tt_ = [k.sb("tt%d" % i, [128, 512], BF16) for i in range(2)]
    yo = [k.sb("yo%d" % i, [128, D], BF16) for i in range(2)]
    pg = [k.ps("pg%d" % i, [128, 512]) for i in range(2)]
    pu = [k.ps("pu%d" % i, [128, 512]) for i in range(2)]
    pb = [k.ps("pb%d" % i, [128, 512]) for i in range(2)]
    py = [k.ps("py%d" % i, [128, 512]) for i in range(2)]
    cnt = dict(hc=0, ett=0, py=0, w=0)
    pending = [None]

    def load_w(e, slot):
        stg.load(Wg[slot], Wg[slot][:], wg[e].rearrange("(kc p) h -> p kc h", p=128))
        stg.load(Wu[slot], Wu[slot][:], wu[e].rearrange("(kc p) h -> p kc h", p=128))
        stg.load(Wd[slot], Wd[slot][:], wd[e].rearrange("(hc p) d -> p hc d", p=128))

    def down(slot, tt, hbuf, first):
        for ts in range(4):
            for dh in range(2):
                pyt = py[cnt['py'] % 2]
                cnt['py'] += 1
                for hc in range(4):
                    k.mm(pyt[:], hbuf[:, hc, ts * 128:(ts + 1) * 128], Wd[slot][:, hc, dh * 512:(dh + 1) * 512],
                         hc == 0, hc == 3, reads=[hbuf, Wd[slot]], writes=[pyt])
                j = tt * 4 + ts
                ya = yacc_h[:, j, dh * 512:(dh + 1) * 512]
                if first:
                    k.op('dve', lambda en: en.tensor_copy(ya, pyt[:]), reads=[pyt], writes=[yacc[j][dh]])
                else:
                    k.op('dve', lambda en: en.tensor_tensor(ya, ya, pyt[:], ALU.add), reads=[pyt, yacc[j][dh]], writes=[yacc[j][dh]])

    def flush():
        if pending[0] is not None:
            pending[0]()
            pending[0] = None

    seq = [(tb, e) for tb in range(n_blocks) for e in range(n_exp)]
    load_w(seq[0][1], 0)
    for si, (tb, e) in enumerate(seq):
        slot = si % 2
        if e == 0:
            flush()
            if tb > 0:
                store_block(k, yacc_h, yacc, yo, yp, tb - 1)
            k.dma('sp', xTb[:], xT[:, tb * TOK:(tb + 1) * TOK].rearrange("(kc p) t -> p kc t", p=128), writes=[xTb])
            k.dma('act', GTb[:], gt[:, tb * TOK:(tb + 1) * TOK], writes=[GTb])
        for tt in range(4):
            pbt = pb[cnt['ett'] % 2]
            hT_t = hT[cnt['ett'] % 2]
            cnt['ett'] += 1
            k.mm(pbt[:], ident[0:EPC, e:e + 1].to_broadcast([EPC, 128]), GTb[:, tt * 512:(tt + 1) * 512], True, True,
                 reads=[ident, GTb], writes=[pbt])
            for hc in range(4):
                b = cnt['hc'] % 2
                cnt['hc'] += 1
                for kc in range(8):
                    k.mm(pg[b][:], Wg[slot][:, kc, hc * 128:(hc + 1) * 128], xTb[:, kc, tt * 512:(tt + 1) * 512],
                         kc == 0, kc == 7, reads=[Wg[slot], xTb], writes=[pg[b]])
                for kc in range(8):
                    k.mm(pu[b][:], Wu[slot][:, kc, hc * 128:(hc + 1) * 128], xTb[:, kc, tt * 512:(tt + 1) * 512],
                         kc == 0, kc == 7, reads=[Wu[slot], xTb], writes=[pu[b]])
                k.op('act', lambda en: en.activation(sg[b][:], pg[b][:], AF.Silu), reads=[pg[b]], writes=[sg[b]])
                k.op('dve', lambda en: en.tensor_tensor(tt_[b][:], sg[b][:], pu[b][:], ALU.mult), reads=[sg[b], pu[b]], writes=[tt_[b]])
                k.op('dve', lambda en: en.tensor_tensor(hT_t[:, hc, :], tt_[b][:], pbt[:], ALU.mult),
                     reads=[tt_[b], pbt], writes=[hT_t])
            flush()
            if tt == 0 and si + 1 < len(seq):
                load_w(seq[si + 1][1], 1 - slot)
            pending[0] = (lambda slot=slot, tt=tt, hT_t=hT_t, first=(e == 0): down(slot, tt, hT_t, first))
    flush()
    store_block(k, yacc_h, yacc, yo, yp, n_blocks - 1)
    k.finish([])
    return nc


def store_block(k, yacc_h, yacc, yo, yp, tb):
    for j in range(NT):
        o = yo[j % 2]
        k.op('act' if j % 2 == 0 else 'pool', lambda en: (en.copy if j % 2 == 0 else en.tensor_copy)(o[:], yacc_h[:, j, :]),
             reads=[yacc[j][0], yacc[j][1]], writes=[o])
        k.dma('sp' if j % 2 == 0 else 'act', yp[tb * TOK + j * 128: tb * TOK + (j + 1) * 128, :], o[:], reads=[o], writes=[])


def build_C():
    nc, k = new_kb()
    x1 = k.dram("x1", [TOK, D], F32, "ExternalInput")
    yp = k.dram("yp", [NCORES, TOK, D], BF16, "ExternalInput")
    lng = k.dram("lng", [D], F32, "ExternalInput")
    lnb = k.dram("lnb", [D], F32, "ExternalInput")
    out = k.dram("out", [TOK, D], F32, "ExternalOutput")
    gb = k.sb("gb", [128, D]); bb = k.sb("bb", [128, D])
    k.dma('sp', gb[:], lng[:].partition_broadcast(128), writes=[gb])
    k.dma('act', bb[:], lnb[:].partition_broadcast(128), writes=[bb])
    xt = [k.sb("c_x%d" % i, [128, D]) for i in range(2)]
    yt = [k.sb("c_y%d" % i, [128, NCORES, D], BF16) for i in range(2)]
    st = [k.sb("st%d" % i, [128, 64]) for i in range(2)]
    for j in range(NT):
        x_ = xt[j % 2]; y_ = yt[j % 2]
        k.dma('sp', x_[:], x1[j * 128:(j + 1) * 128, :], writes=[x_])
        k.dma('act', y_[:], yp[:, j * 128:(j + 1) * 128, :].rearrange("c p d -> p c d"), writes=[y_])
        k.op('dve', lambda e: e.scalar_tensor_tensor(x_[:], x_[:], ALPHA, y_[:, 0, :], ALU.mult, ALU.add), reads=[x_, y_], writes=[x_])
        for c in range(1, NCORES):
            k.op('dve' if c % 2 else 'pool', lambda e: e.tensor_tensor(x_[:], x_[:], y_[:, c, :], ALU.add), reads=[x_, y_], writes=[x_])
        ln_rows(k, x_[:], x_, D, gb[:], bb[:], gb, bb, st[j % 2])
        k.dma('sp', out[j * 128:(j + 1) * 128, :], x_[:], reads=[x_], writes=[])
    k.finish([])
    return nc


GELU_C = 1.5957691216057308


def gelu_tanh(k, out_ap, out_t, ps_t, ps_ap, tmp):
    xs, x2, z = tmp
    k.op('act', lambda e: e.copy(xs[:], ps_ap), reads=[ps_t], writes=[xs])
    k.op('dve', lambda e: e.tensor_tensor(x2[:], xs[:], xs[:], ALU.mult), reads=[xs], writes=[x2])
    k.op('dve', lambda e: e.tensor_scalar(x2[:], x2[:], 0.044715, 1.0, ALU.mult, ALU.add), reads=[x2], writes=[x2])
    k.op('pool', lambda e: e.tensor_tensor(z[:], x2[:], xs[:], ALU.mult), reads=[x2, xs], writes=[z])
    k.op('act', lambda e: e.activation(z[:], z[:], AF.Sigmoid, scale=GELU_C), reads=[z], writes=[z])
    k.op('dve', lambda e: e.tensor_tensor(out_ap, xs[:], z[:], ALU.mult), reads=[xs, z], writes=[out_t])


def load_xT(k, xT, x_src, ident, n_tiles, pT, tok0=0):
    with k.scope():
        xt = [k.sb("lx%d" % i, [128, D]) for i in range(2)]
        for j in range(n_tiles):
            x_ = xt[j % 2]
            k.dma('sp' if j % 2 == 0 else 'act', x_[:], x_src[j * 128:(j + 1) * 128, :], writes=[x_])
            for hf in range(2):
                pt = pT[(2 * j + hf) % len(pT)]
                for kk in range(4):
                    kc = hf * 4 + kk
                    k.op('pe', lambda e: e.transpose(pt[:, kk * 128:(kk + 1) * 128], x_[:, kc * 128:(kc + 1) * 128], ident[:]),
                         reads=[x_, ident], writes=[pt])
                k.op('act' if hf == 0 else 'dve',
                     lambda e: (e.copy if hf == 0 else e.tensor_copy)(xT[:, hf * 4:hf * 4 + 4, tok0 + j * 128:tok0 + (j + 1) * 128],
                                                                     pt[:].rearrange("p (k t) -> p k t", k=4)),
                     reads=[pt], writes=[xT])


def declare_post_io(k, fused=True):
    nc = k.nc
    io = dict(
        lng=k.dram("lng", [D], F32, "ExternalInput"), lnb=k.dram("lnb", [D], F32, "ExternalInput"),
        wr=k.dram("wr", [D, 72], F32, "ExternalInput"), idn=k.dram("idn", [128, 128], F32, "ExternalInput"))
    if not fused:
        io.update(x1o=k.dram("x1o", [TOK, D], F32, "ExternalOutput"), x1T=k.dram("x1T", [D, TOK], BF16, "ExternalOutput"),
                  gto=k.dram("gto", [64, TOK], F32, "ExternalOutput"))
    else:
        io.update(x1o=T(nc.dram_tensor("x1o_scr", [TOK, D], F32), "x1o_scr"), x1T=T(nc.dram_tensor("x1T_scr", [D, TOK], BF16), "x1T_scr"),
                  gto=T(nc.dram_tensor("gt_scr", [64, TOK], F32), "gt_scr"),
                  wg=k.dram("wg", [64, D, HID], F32, "ExternalInput"), wu=k.dram("wu", [64, D, HID], F32, "ExternalInput"),
                  wd=k.dram("wd", [64, HID, D], F32, "ExternalInput"),
                  lng2=k.dram("lng2", [D], F32, "ExternalInput"), lnb2=k.dram("lnb2", [D], F32, "ExternalInput"),
                  x2=k.dram("x2", [TOK, D], F32, "ExternalOutput"))
    io['fused'] = fused
    return io


def tail(k, io):
    if io['fused']:
        moe_tp(k, io)
    k.finish([])
    return k.nc


def moe_tp(k, io, n_exp=64):
    nc = k.nc
    wg, wu, wd = io['wg'], io['wu'], io['wd']
    with k.scope():
        ident = k.sb("e_ident", [128, 128])
        k.dma('sp', ident[:], io['idn'][:], writes=[ident])
        yacc_t = k.sb("e_yacc", [128, NT, D])
        yacc_h = yacc_t.h
        yacc = [[T(yacc_h, "yacc%d_%d" % (j, h)) for h in range(2)] for j in range(NT)]
        yfull = [T(yacc_h, "yaccf%d" % j) for j in range(NT)]
        xTb = k.sb("e_xTb", [128, 8, TOK], BF16)
        GTb = k.sb("e_GTb", [64, TOK])
        Wg = [k.sb("e_Wg%d" % i, [128, 8, HID], BF16) for i in range(2)]
        Wu = [k.sb("e_Wu%d" % i, [128, 8, HID], BF16) for i in range(2)]
        Wd = [k.sb("e_Wd%d" % i, [128, 4, D], BF16) for i in range(2)]
        stg = Stager(k, 2, 4096)
        hT = [k.sb("e_hT%d" % i, [128, 4, 512], BF16) for i in range(2)]
        sg = [k.sb("e_sg%d" % i, [128, 512], BF16) for i in range(2)]
        tt_ = [k.sb("e_tt%d" % i, [128, 512], BF16) for i in range(2)]
        pg = [k.ps("e_pg%d" % i, [128, 512]) for i in range(2)]
        pu = [k.ps("e_pu%d" % i, [128, 512]) for i in range(2)]
        pb = [k.ps("e_pb%d" % i, [128, 512]) for i in range(2)]
        py = [k.ps("e_py%d" % i, [128, 512]) for i in range(2)]
        cnt = dict(hc=0, ett=0, py=0)
        pending = [None]
        k.dma('sp', xTb[:], io['x1T'][:, :].rearrange("(kc p) t -> p kc t", p=128), writes=[xTb])
        k.dma('act', GTb[:], io['gto'][:, :], writes=[GTb])
        for j in range(NT):
            k.dma('sp' if j % 2 == 0 else 'act', yacc_h[:, j, :], io['x1o'][j * 128:(j + 1) * 128, :], writes=[yfull[j]])
            k.op('pool', lambda e: e.tensor_scalar(yacc_h[:, j, :], yacc_h[:, j, :], ALPHA, None, ALU.mult), reads=[yfull[j]], writes=[yfull[j]])
        k.barrier()

        def load_w(e, slot):
            stg.load(Wg[slot], Wg[slot][:], wg[e].rearrange("(kc p) h -> p kc h", p=128))
            stg.load(Wu[slot], Wu[slot][:], wu[e].rearrange("(kc p) h -> p kc h", p=128))
            stg.load(Wd[slot], Wd[slot][:], wd[e].rearrange("(hc p) d -> p hc d", p=128))

        def down(slot, tt, hbuf):
            for ts in range(4):
                for dh in range(2):
                    pyt = py[cnt['py'] % 2]
                    cnt['py'] += 1
                    for hc in range(4):
                        k.mm(pyt[:], hbuf[:, hc, ts * 128:(ts + 1) * 128], Wd[slot][:, hc, dh * 512:(dh + 1) * 512],
                             hc == 0, hc == 3, reads=[hbuf, Wd[slot]], writes=[pyt])
                    j = tt * 4 + ts
                    ya = yacc_h[:, j, dh * 512:(dh + 1) * 512]
                    k.op('dve', lambda en: en.tensor_tensor(ya, ya, pyt[:], ALU.add), reads=[pyt, yacc[j][dh]], writes=[yacc[j][dh]])

        def flush():
            if pending[0] is not None:
                pending[0]()
                pending[0] = None

        load_w(0, 0)
        for e in range(n_exp):
            slot = e % 2
            for tt in range(4):
                pbt = pb[cnt['ett'] % 2]
                hT_t = hT[cnt['ett'] % 2]
                cnt['ett'] += 1
                k.mm(pbt[:], ident[0:64, e:e + 1].to_broadcast([64, 128]), GTb[:, tt * 512:(tt + 1) * 512], True, True,
                     reads=[ident, GTb], writes=[pbt])
                for hc in range(4):
                    b = cnt['hc'] % 2
                    cnt['hc'] += 1
                    for kc in range(8):
                        k.mm(pg[b][:], Wg[slot][:, kc, hc * 128:(hc + 1) * 128], xTb[:, kc, tt * 512:(tt + 1) * 512],
                             kc == 0, kc == 7, reads=[Wg[slot], xTb], writes=[pg[b]])
                    for kc in range(8):
                        k.mm(pu[b][:], Wu[slot][:, kc, hc * 128:(hc + 1) * 128], xTb[:, kc, tt * 512:(tt + 1) * 512],
                             kc == 0, kc == 7, reads=[Wu[slot], xTb], writes=[pu[b]])
                    k.op('act', lambda en: en.activation(sg[b][:], pg[b][:], AF.Silu), reads=[pg[b]], writes=[sg[b]])
                    k.op('dve', lambda en: en.tensor_tensor(tt_[b][:], sg[b][:], pu[b][:], ALU.mult), reads=[sg[b], pu[b]], writes=[tt_[b]])
                    k.op('dve', lambda en: en.tensor_tensor(hT_t[:, hc, :], tt_[b][:], pbt[:], ALU.mult),
                         reads=[tt_[b], pbt], writes=[hT_t])
                flush()
                if tt == 0 and e + 1 < n_exp:
                    load_w(e + 1, 1 - slot)
                pending[0] = (lambda slot=slot, tt=tt, hT_t=hT_t: down(slot, tt, hT_t))
        flush()
        k.barrier()
        gbf = Wg[0]; bbf = Wu[0]
        gbv = gbf.h[:].rearrange("p k h -> p (k h)").bitcast(F32)[:, 0:D]
        bbv = bbf.h[:].rearrange("p k h -> p (k h)").bitcast(F32)[:, 0:D]
        k.dma('sp', gbv, io['lng2'][:].partition_broadcast(128), writes=[gbf])
        k.dma('act', bbv, io['lnb2'][:].partition_broadcast(128), writes=[bbf])
        st = [k.sb("e_st%d" % i, [128, 64]) for i in range(2)]
        for j in range(NT):
            ln_rows(k, yacc_h[:, j, :], yfull[j], D, gbv, bbv, gbf, bbf, st[j % 2])
            k.dma('sp' if j % 2 == 0 else 'act', io['x2'][j * 128:(j + 1) * 128, :], yacc_h[:, j, :], reads=[yfull[j]], writes=[])


def build_M_sgu(n_chunks=NT, fused=True):
    nc, k = new_kb()
    x = k.dram("x", [TOK, D], F32, "ExternalInput")
    w_in = k.dram("w_in", [D, 4096], F32, "ExternalInput")
    ng = k.dram("ng", [2048], F32, "ExternalInput")
    nb = k.dram("nb", [2048], F32, "ExternalInput")
    w_s = k.dram("w_s", [16, 128, 128], F32, "ExternalInput")
    b_s = k.dram("b_s", [2048], F32, "ExternalInput")
    w_out = k.dram("w_out", [2048, D], F32, "ExternalInput")
    io = declare_post_io(k, fused)
    hs = T(nc.dram_tensor("h_scr", [TOK, D], F32), "h_scr")
    with k.scope():
        ident = k.sb("ident", [128, 128])
        k.dma('sp', ident[:], io['idn'][:], writes=[ident])
        xT = k.sb("xT", [128, 8, TOK], BF16)
        w_inb = k.sb("w_inb", [128, 8, 4096], BF16)
        w_outb = k.sb("w_outb", [128, 16, D], BF16)
        WcT = k.sb("WcT", [128, 16, 128], BF16)
        ngb = k.sb("ngb", [128, 2048]); nbb = k.sb("nbb", [128, 2048]); bsb = k.sb("bsb", [128, 2048])
        pv = [k.ps("pv%d" % i, [128, 512]) for i in range(2)]
        pu = [k.ps("pu%d" % i, [128, 512]) for i in range(2)]
        pf = [k.ps("pf%d" % i, [128, 512]) for i in range(2)]
        ph = [k.ps("ph%d" % i, [128, 512]) for i in range(2)]
        k.dma('sp', ngb[:], ng[:].partition_broadcast(128), writes=[ngb])
        k.dma('act', nbb[:], nb[:].partition_broadcast(128), writes=[nbb])
        k.dma('sp', bsb[:], b_s[:].partition_broadcast(128), writes=[bsb])
        with k.scope():
            stg = Stager(k, 2, 4096)
            stg.load(w_inb, w_inb[:], w_in[:].rearrange("(kc p) n -> p kc n", p=128))
            stg.load(w_outb, w_outb[:], w_out[:].rearrange("(cc p) d -> p cc d", p=128))
            ws = k.sb("ws", [128, 16, 128])
            k.dma('sp', ws[:], w_s[:].rearrange("g t s -> t g s"), writes=[ws])
            k.op('pool', lambda e: e.affine_select(ws[:], ws[:], [[0, 16], [-1, 128]], ALU.is_ge, 0.0, base=0, channel_multiplier=1),
                 reads=[ws], writes=[ws])
            for g4 in range(4):
                pt = pv[g4 % 2]
                for gg in range(4):
                    g = g4 * 4 + gg
                    k.op('pe', lambda e: e.transpose(pt[:, gg * 128:(gg + 1) * 128], ws[:, g, :], ident[:]), reads=[ws, ident], writes=[pt])
                k.op('act', lambda e: e.copy(WcT[:, g4 * 4:g4 * 4 + 4, :], pt[:].rearrange("p (g t) -> p g t", g=4)), reads=[pt], writes=[WcT])
        load_xT(k, xT, x, ident, NT, pu + pf)
        v = k.sb("v", [128, 2048]); vb = k.sb("vb", [128, 2048], BF16)
        uT = k.sb("uT", [128, 16, 128], BF16); ufT = k.sb("ufT", [128, 16, 128], BF16)
        t1 = k.sb("t1", [128, 512])
        tmp = [k.sb("gt%d" % i, [128, 512]) for i in range(3)]
        st = k.sb("st", [128, 64])
        ht = [k.sb("ht%d" % i, [128, D]) for i in range(2)]
        for n in range(n_chunks):
            tok = slice(n * 128, (n + 1) * 128)
            for cb in range(4):
                p_ = pv[cb % 2]
                for kc in range(8):
                    k.mm(p_[:], xT[:, kc, tok], w_inb[:, kc, 2048 + cb * 512:2048 + (cb + 1) * 512], kc == 0, kc == 7,
                         reads=[xT, w_inb], writes=[p_])
                gelu_tanh(k, v[:, cb * 512:(cb + 1) * 512], v, p_, p_[:], tmp)
            ln_rows(k, v[:], v, 2048, ngb[:], nbb[:], ngb, nbb, st)
            k.op('pool', lambda e: e.tensor_copy(vb[:], v[:]), reads=[v], writes=[vb])
            for c4 in range(4):
                p_ = pu[c4 % 2]
                for cc in range(4):
                    c = c4 * 4 + cc
                    for kc in range(8):
                        k.mm(p_[:, cc * 128:(cc + 1) * 128], w_inb[:, kc, c * 128:(c + 1) * 128], xT[:, kc, tok], kc == 0, kc == 7,
                             reads=[xT, w_inb], writes=[p_])
                gelu_tanh(k, uT[:, c4 * 4:c4 * 4 + 4, :].rearrange("p c t -> p (c t)"), uT, p_, p_[:], tmp)
                q_ = pf[c4 % 2]
                for cc in range(4):
                    c = c4 * 4 + cc
                    k.mm(q_[:, cc * 128:(cc + 1) * 128], vb[:, c * 128:(c + 1) * 128], WcT[:, c, :], True, True,
                         reads=[vb, WcT], writes=[q_])
                k.op('dve', lambda e: e.tensor_tensor(t1[:].rearrange("p (c t) -> p c t", c=4), q_[:].rearrange("p (c t) -> p c t", c=4),
                                                      bsb[:, c4 * 512:(c4 + 1) * 512].rearrange("p (c t) -> p c t", c=4), ALU.add),
                     reads=[q_, bsb], writes=[t1])
                k.op('dve', lambda e: e.tensor_tensor(ufT[:, c4 * 4:c4 * 4 + 4, :].rearrange("p c t -> p (c t)"), t1[:],
                                                      uT[:, c4 * 4:c4 * 4 + 4, :].rearrange("p c t -> p (c t)"), ALU.mult),
                     reads=[t1, uT], writes=[ufT])
            h_ = ht[n % 2]
            for dh in range(2):
                for c in range(16):
                    k.mm(ph[dh][:], ufT[:, c, :], w_outb[:, c, dh * 512:(dh + 1) * 512], c == 0, c == 15, reads=[ufT, w_outb], writes=[ph[dh]])
                k.op('act', lambda e: e.copy(h_[:, dh * 512:(dh + 1) * 512], ph[dh][:]), reads=[ph[dh]], writes=[h_])
            k.dma('sp', hs[n * 128:(n + 1) * 128, :], h_[:], reads=[h_], writes=[hs])
    post(k, x, hs, io['lng'], io['lnb'], io['wr'], io['idn'], io['x1o'], io['x1T'], io['gto'])
    return tail(k, io)


A_GROUPS = ((128, 1), (512, 4), (2048, 16))
NEG = -30000.0
HQ = 4


def build_M_att(fused=True):
    nc, k = new_kb()
    x = k.dram("x", [TOK, D], F32, "ExternalInput")
    xh = k.dram("xh", [TOK, D], F32, "ExternalInput")
    w_in = k.dram("w_in", [D, 9216], F32, "ExternalInput")
    w_out = k.dram("w_out", [D, D], F32, "ExternalInput")
    tab = k.dram("tab", [3, 16, 128, 256], F32, "ExternalInput")
    negf = k.dram("negf", [128, 128], F32, "ExternalInput")
    io = declare_post_io(k, fused)
    hs = T(nc.dram_tensor("h_scr", [TOK, D], F32), "h_scr")
    Og = [T(nc.dram_tensor("O_scr%d" % g, [TOK, 16, 64], F32), "O_scr%d" % g) for g in range(3)]
    Lg = [T(nc.dram_tensor("L_scr%d" % g, [TOK, 16], F32), "L_scr%d" % g) for g in range(3)]
    with k.scope():
        ident = k.sb("ident", [128, 128])
        identb = k.sb("identb", [128, 128], BF16)
        k.dma('sp', ident[:], io['idn'][:], writes=[ident])
        k.op('dve', lambda e: e.tensor_copy(identb[:], ident[:]), reads=[ident], writes=[identb])
        xT = k.sb("xT", [128, 8, 2 * TOK], BF16)
        pq = [k.ps("pq%d" % i, [128, 512]) for i in range(2)]
        NU = 3
        pSO = [k.ps("pSO%d" % i, [128, 512]) for i in range(NU)]
        pP = [k.ps("pP%d" % i, [128, 256], BF16) for i in range(NU)]
        load_xT(k, xT, xh, ident, NT, pq + pSO[0:2], tok0=0)
        load_xT(k, xT, x, ident, NT, pq + pSO[0:2], tok0=TOK)
        stg = Stager(k, 2, 2048)
        Wq = k.sb("Wq", [128, 8, HQ * 64], BF16); Wk = k.sb("Wk", [128, 8, HQ * 64], BF16); Wv = k.sb("Wv", [128, 8, HQ * 64], BF16)
        qT = k.sb("qT", [128, HQ // 2, TOK], BF16)
        kT = k.sb("kT", [128, HQ // 2, 2 * TOK], BF16)
        vv = k.sb("vv", [128, 32, HQ * 64], BF16)
        tb = k.sb("tb", [128, HQ, 256]); tb0 = k.sb("tb0", [128, HQ, 256])
        ngf = k.sb("ngf", [128, 128])
        k.dma('sp', ngf[:], negf[:], writes=[ngf])
        s_ = [k.sb("s%d" % i, [128, 256]) for i in range(NU)]
        p_ = [k.sb("p%d" % i, [128, 256], BF16) for i in range(NU)]
        PT = [k.sb("PT%d" % i, [128, 256], BF16) for i in range(NU)]
        sm = [k.sb("sm%d" % i, [128, 8]) for i in range(NU)]
        Ob = [k.sb("Ob%d" % i, [128, HQ, 64]) for i in range(2)]
        Lb = [k.sb("Lb%d" % i, [128, HQ]) for i in range(2)]
        cnt = dict(q=0, u=0, blk=0)
        for g, (window, d) in enumerate(A_GROUPS):
            n_own = TOK // d
            n_all = 2 * TOK // d
            nkb = n_all // 128
            nb = n_own // 128
            for hq in range(16 // HQ):
                c0 = hq * HQ * 64
                for j, W in enumerate((Wq, Wk, Wv)):
                    base = (g * 3 + j) * 1024 + c0
                    stg.load(W, W[:], w_in[:, base:base + HQ * 64].rearrange("(kc p) n -> p kc n", p=128))
                k.dma('sp', tb[:], tab[g, hq * HQ:(hq + 1) * HQ].rearrange("h q c -> q h c"), writes=[tb])
                k.op('pool', lambda e: e.tensor_copy(tb0[:, :, 128:256], tb[:, :, 128:256]), reads=[tb], writes=[tb0])
                k.op('pool', lambda e: e.tensor_tensor(tb0[:, :, 0:128], tb[:, :, 0:128], ngf[:].unsqueeze(1).to_broadcast([128, HQ, 128]), ALU.add),
                     reads=[tb, ngf], writes=[tb0])
                for r in range(d):
                    for (dst, W, lo, n) in ((qT, Wq, TOK + r, n_own), (kT, Wk, r, n_all)):
                        ch = min(512, n)
                        for c in range(n // ch):
                            for pp in range(HQ // 2):
                                pt = pq[cnt['q'] % 2]; cnt['q'] += 1
                                a = lo + d * c * ch
                                for kc in range(8):
                                    k.mm(pt[:, 0:ch], W[:, kc, pp * 128:(pp + 1) * 128], xT[:, kc, a:a + d * (ch - 1) + 1:d], kc == 0, kc == 7,
                                         reads=[W, xT], writes=[pt])
                                col = r * n + c * ch
                                k.op('act' if cnt['q'] % 2 else 'dve',
                                     lambda e: (e.copy if cnt['q'] % 2 else e.tensor_copy)(dst[:, pp, col:col + ch], pt[:, 0:ch]),
                                     reads=[pt], writes=[dst])
                    for kb in range(nkb):
                        pt = pq[cnt['q'] % 2]; cnt['q'] += 1
                        a = r + d * kb * 128
                        for kc in range(8):
                            k.mm(pt[:, 0:HQ * 64], xT[:, kc, a:a + d * 127 + 1:d], Wv[:, kc, :], kc == 0, kc == 7, reads=[Wv, xT], writes=[pt])
                        k.op('act' if cnt['q'] % 2 else 'dve',
                             lambda e: (e.copy if cnt['q'] % 2 else e.tensor_copy)(vv[:, r * nkb + kb, :], pt[:, 0:HQ * 64]),
                             reads=[pt], writes=[vv])
                units = [(r, b, h) for r in range(d) for b in range(nb) for h in range(HQ)]
                nun = len(units)
                blk0 = cnt['blk']
                cnt['blk'] += nun // HQ

                def st1(i):
                    r, b, h = units[i]
                    u = i % NU
                    pp, hi = h // 2, h % 2
                    tbl = tb0 if b == 0 else tb
                    qa = qT[hi * 64:(hi + 1) * 64, pp, r * n_own + b * 128:r * n_own + (b + 1) * 128]
                    kc0 = r * n_all + n_own + 128 * (b - 1)
                    ka = kT[hi * 64:(hi + 1) * 64, pp, kc0:kc0 + 256]
                    k.mm(pSO[u][:, 0:256], qa, ka, True, True, reads=[qT, kT], writes=[pSO[u]])
                    k.op('dve', lambda e: e.scalar_tensor_tensor(s_[u][:], pSO[u][:, 0:256], 0.125, tbl[:, h, :], ALU.mult, ALU.add),
                         reads=[pSO[u], tbl], writes=[s_[u]])
                    k.op('dve', lambda e: e.tensor_reduce(sm[u][:, 0:1], s_[u][:], AX.X, ALU.max), reads=[s_[u]], writes=[sm[u]])
                    k.op('dve', lambda e: e.tensor_scalar(sm[u][:, 1:2], sm[u][:, 0:1], -1.0, None, ALU.mult), reads=[sm[u]], writes=[sm[u]])
                    k.op('act', lambda e: e.activation(p_[u][:], s_[u][:], AF.Exp, bias=sm[u][:, 1:2], scale=1.0, accum_out=sm[u][:, 2:3]),
                         reads=[s_[u], sm[u]], writes=[p_[u], sm[u]])

                def st2(i):
                    u = i % NU
                    for half in range(2):
                        k.op('pe', lambda e: e.transpose(pP[u][:, half * 128:(half + 1) * 128], p_[u][:, half * 128:(half + 1) * 128], identb[:]),
                             reads=[p_[u], identb], writes=[pP[u]])
                    k.op('act', lambda e: e.copy(PT[u][:], pP[u][:]), reads=[pP[u]], writes=[PT[u]])

                def st3(i):
                    r, b, h = units[i]
                    u = i % NU
                    ob = Ob[(blk0 + i // HQ) % 2]; lb = Lb[(blk0 + i // HQ) % 2]
                    kb0 = n_own // 128 + b - 1
                    for half in range(2):
                        k.mm(pSO[u][:, 256:320], PT[u][:, half * 128:(half + 1) * 128], vv[:, r * nkb + kb0 + half, h * 64:(h + 1) * 64],
                             half == 0, half == 1, reads=[PT[u], vv], writes=[pSO[u]])
                    k.op('dve', lambda e: e.reciprocal(sm[u][:, 3:4], sm[u][:, 2:3]), reads=[sm[u]], writes=[sm[u]])
                    k.op('dve', lambda e: e.tensor_scalar(ob[:, h, :], pSO[u][:, 256:320], sm[u][:, 3:4], None, ALU.mult),
                         reads=[pSO[u], sm[u]], writes=[ob])
                    k.op('act', lambda e: e.activation(sm[u][:, 4:5], sm[u][:, 2:3], AF.Ln), reads=[sm[u]], writes=[sm[u]])
                    k.op('dve', lambda e: e.tensor_tensor(lb[:, h:h + 1], sm[u][:, 4:5], sm[u][:, 0:1], ALU.add), reads=[sm[u]], writes=[lb])
                    if h == HQ - 1:
                        row0 = r + d * 128 * b
                        rows = slice(row0, row0 + d * 127 + 1, d)
                        k.dma('sp', Og[g][rows, hq * HQ:(hq + 1) * HQ, :], ob[:], reads=[ob], writes=[Og[g]])
                        k.dma('act', Lg[g][rows, hq * HQ:(hq + 1) * HQ], lb[:], reads=[lb], writes=[Lg[g]])

                for i in range(nun + 2):
                    if i < nun:
                        st1(i)
                    if 0 <= i - 1 < nun:
                        st2(i - 1)
                    if 0 <= i - 2 < nun:
                        st3(i - 2)
    with k.scope():
        ident = k.sb("m_ident", [128, 128])
        k.dma('sp', ident[:], io['idn'][:], writes=[ident])
        w_outb = k.sb("m_wout", [128, 8, D], BF16)
        with k.scope():
            stg = Stager(k, 2, 4096)
            stg.load(w_outb, w_outb[:], w_out[:].rearrange("(kc p) d -> p kc d", p=128))
        O3 = [k.sb("m_O%d" % i, [128, 3, 16, 64]) for i in range(2)]
        L3 = [k.sb("m_L%d" % i, [128, 3, 16]) for i in range(2)]
        wk = [k.sb("m_w%d" % i, [128, 8, 16]) for i in range(2)]
        om = [k.sb("m_om%d" % i, [128, 16, 64]) for i in range(2)]
        ot = [k.sb("m_ot%d" % i, [128, 16, 64]) for i in range(2)]
        oT = [k.sb("m_oT%d" % i, [128, 8, 128], BF16) for i in range(2)]
        ht = [k.sb("m_h%d" % i, [128, D]) for i in range(2)]
        pT = [k.ps("m_pT%d" % i, [128, 512]) for i in range(2)]
        ph = [k.ps("m_ph%d" % i, [128, 512]) for i in range(2)]
        for j in range(NT):
            i = j % 2
            rows = slice(j * 128, (j + 1) * 128)
            for g in range(3):
                k.dma('sp', O3[i][:, g, :, :], Og[g][rows, :, :], reads=[Og[g]], writes=[O3[i]])
                k.dma('act', L3[i][:, g, :], Lg[g][rows, :], reads=[Lg[g]], writes=[L3[i]])
            w = wk[i]
            k.op('dve', lambda e: e.tensor_reduce(w[:, 0, :], L3[i][:].rearrange("p g h -> p h g"), AX.X, ALU.max), reads=[L3[i]], writes=[w])
            k.op('dve', lambda e: e.tensor_tensor(w[:, 1:4, :], L3[i][:], w[:, 0:1, :].to_broadcast([128, 3, 16]), ALU.subtract), reads=[L3[i], w], writes=[w])
            k.op('act', lambda e: e.activation(w[:, 1:4, :], w[:, 1:4, :], AF.Exp), reads=[w], writes=[w])
            k.op('dve', lambda e: e.tensor_reduce(w[:, 4, :], w[:, 1:4, :].rearrange("p g h -> p h g"), AX.X, ALU.add), reads=[w], writes=[w])
            k.op('dve', lambda e: e.reciprocal(w[:, 4, :], w[:, 4, :]), reads=[w], writes=[w])
            k.op('dve', lambda e: e.tensor_tensor(w[:, 5:8, :], w[:, 1:4, :], w[:, 4:5, :].to_broadcast([128, 3, 16]), ALU.mult), reads=[w], writes=[w])
            o_ = om[i]; t_ = ot[i]
            k.op('dve', lambda e: e.tensor_tensor(o_[:], O3[i][:, 0, :, :], w[:, 5, :].unsqueeze(2).to_broadcast([128, 16, 64]), ALU.mult), reads=[O3[i], w], writes=[o_])
            for g in (1, 2):
                k.op('pool', lambda e: e.tensor_tensor(t_[:], O3[i][:, g, :, :], w[:, 5 + g, :].unsqueeze(2).to_broadcast([128, 16, 64]), ALU.mult), reads=[O3[i], w], writes=[t_])
                k.op('dve', lambda e: e.tensor_tensor(o_[:], o_[:], t_[:], ALU.add), reads=[o_, t_], writes=[o_])
            of = o_[:].rearrange("p h c -> p (h c)")
            for hf in range(2):
                pt = pT[hf]
                for kk in range(4):
                    kc = hf * 4 + kk
                    k.op('pe', lambda e: e.transpose(pt[:, kk * 128:(kk + 1) * 128], of[:, kc * 128:(kc + 1) * 128], ident[:]), reads=[o_, ident], writes=[pt])
                k.op('act', lambda e: e.copy(oT[i][:, hf * 4:hf * 4 + 4, :], pt[:].rearrange("p (k t) -> p k t", k=4)), reads=[pt], writes=[oT[i]])
            h_ = ht[i]
            for dh in range(2):
                for kc in range(8):
                    k.mm(ph[dh][:], oT[i][:, kc, :], w_outb[:, kc, dh * 512:(dh + 1) * 512], kc == 0, kc == 7, reads=[oT[i], w_outb], writes=[ph[dh]])
                k.op('act', lambda e: e.copy(h_[:, dh * 512:(dh + 1) * 512], ph[dh][:]), reads=[ph[dh]], writes=[h_])
            k.dma('sp', hs[rows, :], h_[:], reads=[h_], writes=[hs])
    post(k, x, hs, io['lng'], io['lnb'], io['wr'], io['idn'], io['x1o'], io['x1T'], io['gto'])
    return tail(k, io)


def t5_bucket_np(dist):
    dist = np.asarray(dist)
    dd = np.maximum(dist, 1).astype(np.float32)
    large = 16 + (np.log(dd / np.float32(16)) / np.float32(math.log(2048 / 16)) * np.float32(16)).astype(np.int32)
    return np.where(dist < 16, dist, np.minimum(large, 31))


def att_tables(rel_bias):
    qi = np.arange(128)[:, None]
    ki = np.arange(256)[None, :]
    rel = qi + 128 - ki
    valid = (rel >= 0) & (rel <= 128)
    tabs = np.full((3, 16, 128, 256), NEG, np.float32)
    for g, (window, d) in enumerate(A_GROUPS):
        bk = t5_bucket_np(np.maximum(rel, 0) * d)
        b = rel_bias[bk]
        tabs[g] = np.where(valid[None], b.transpose(2, 0, 1), np.float32(NEG))
    return tabs


C_H = 8
CH = 64
SC = 128
RMS_EPS = 1e-6


def build_M_gdn(n_sc=2 * TOK // SC, fused=True):
    nc, k = new_kb()
    x = k.dram("x", [TOK, D], F32, "ExternalInput")
    xh = k.dram("xh", [TOK, D], F32, "ExternalInput")
    w_in = k.dram("w_in", [D, 4112], F32, "ExternalInput")
    conv = k.dram("conv", [4, 3072], F32, "ExternalInput")
    a_log = k.dram("a_log", [8], F32, "ExternalInput")
    dt_b = k.dram("dt_b", [8], F32, "ExternalInput")
    norm_w = k.dram("norm_w", [128], F32, "ExternalInput")
    w_out = k.dram("w_out", [D, D], F32, "ExternalInput")
    io = declare_post_io(k, fused)
    hs = T(nc.dram_tensor("h_scr", [TOK, D], F32), "h_scr")
    scale = 128 ** -0.5
    with k.scope():
        B = [k.ps("B%d" % i, [128, 512]) for i in range(8)]
        ident = k.sb("ident", [128, 128])
        k.dma('sp', ident[:], io['idn'][:], writes=[ident])
        ones = k.sb("ones", [128, 128])
        k.op('dve', lambda e: e.memset(ones[:], 1.0), writes=[ones])
        w_inb = k.sb("w_inb", [128, 8, 4112], BF16)
        w_outb = k.sb("w_outb", [128, 8, D], BF16)
        cwT = k.sb("cwT", [128, 4, 24])
        with k.scope():
            stg = Stager(k, 2, 4112)
            stg.load(w_inb, w_inb[:], w_in[:].rearrange("(kc p) n -> p kc n", p=128))
            stg.load(w_outb, w_outb[:], w_out[:].rearrange("(kc p) d -> p kc d", p=128))
            ct = k.sb("ct", [24, 4, 128])
            k.dma('sp', ct[:], conv[:].rearrange("j (fb p) -> fb j p", p=128), writes=[ct])
            for j in range(4):
                k.op('pe', lambda e: e.transpose(B[0][:, j * 24:(j + 1) * 24], ct[:, j, :], ident[0:24, 0:24]), reads=[ct, ident], writes=[B[0]])
            k.op('act', lambda e: e.copy(cwT[:].rearrange("p j f -> p (j f)"), B[0][:, 0:96]), reads=[B[0]], writes=[cwT])
        mLs = k.sb("mLs", [CH, 8, CH]); mUs = k.sb("mUs", [CH, 8, CH]); mUi = k.sb("mUi", [CH, 8, CH]); triU = k.sb("triU", [CH, CH])
        for (m, cmp_, pat, cm) in ((mLs, ALU.is_gt, [[0, 8], [-1, CH]], 1), (mUs, ALU.is_gt, [[0, 8], [1, CH]], -1),
                                   (mUi, ALU.is_ge, [[0, 8], [1, CH]], -1)):
            k.op('pool', lambda e: e.memset(m[:], 1.0), writes=[m])
            k.op('pool', lambda e: e.affine_select(m[:], m[:], pat, cmp_, 0.0, base=0, channel_multiplier=cm), reads=[m], writes=[m])
        k.op('pool', lambda e: e.memset(triU[:], 1.0), writes=[triU])
        k.op('pool', lambda e: e.affine_select(triU[:], triU[:], [[1, CH]], ALU.is_ge, 0.0, base=0, channel_multiplier=-1), reads=[triU], writes=[triU])
        cst = k.sb("cst", [CH, 32])
        k.dma('sp', cst[:, 0:8], dt_b[:].partition_broadcast(CH), writes=[cst])
        k.dma('sp', cst[:, 8:16], a_log[:].partition_broadcast(CH), writes=[cst])
        k.op('act', lambda e: e.activation(cst[:, 8:16], cst[:, 8:16], AF.Exp), reads=[cst], writes=[cst])
        k.op('dve', lambda e: e.tensor_scalar(cst[:, 8:16], cst[:, 8:16], -1.0, None, ALU.mult), reads=[cst], writes=[cst])
        nwb = k.sb("nwb", [CH, 128])
        k.dma('sp', nwb[:], norm_w[:].partition_broadcast(CH), writes=[nwb])
        xt = [k.sb("gx%d" % i, [128, D]) for i in range(2)]
        xTs = k.sb("xTs", [128, 8, SC], BF16)
        pre = k.sb("pre", [128, 24, SC + 3]); tl = k.sb("tl", [128, 24, 3])
        k.op('dve', lambda e: e.memset(pre[:], 0.0), writes=[pre])
        cv = k.sb("cv", [128, 24, SC]); cv2 = k.sb("cv2", [128, 24, SC])
        sq = k.sb("sq", [128, 16, CH]); rinv = k.sb("rinv", [128, 16, CH]); qkn = k.sb("qkn", [128, 16, CH])
        ktm = k.sb("ktm", [CH, 8, 128]); vtm = k.sb("vtm", [CH, 8, 128]); kd = k.sb("kd", [CH, 8, 128])
        sm = k.sb("sm", [CH, 16, 8])
        sm128 = k.sb("sm128", [128, 16])
        dg = k.sb("dg", [CH, 8, CH]); db = k.sb("db", [CH, 8, CH])
        E1 = k.sb("E1", [CH, 8, CH]); E2 = k.sb("E2", [CH, 8, CH]); E3 = k.sb("E3", [CH, 8, CH])
        A = k.sb("A", [CH, 8, CH]); AT = k.sb("AT", [CH, 8, CH]); IT = k.sb("IT", [CH, 8, CH])
        X = k.sb("X", [CH, 8, 256])
        kcT = k.sb("kcT", [128, 8, CH])
        S = k.sb("S", [128, 8, 128])
        k.op('dve', lambda e: e.memset(S[:], 0.0), writes=[S] + [])
        k.barrier()
        vn = [k.sb("vn%d" % i, [CH, 128]) for i in range(2)]; qs = [k.sb("qs%d" % i, [CH, 128]) for i in range(2)]
        S_t = [T(S.h, "S_h%d" % i) for i in range(8)]
        o = k.sb("o", [CH, 8, 128]); zs = k.sb("zs", [CH, 8, 128])
        ogT = k.sb("ogT", [128, 8, CH], BF16)
        ht = k.sb("ht", [CH, D])
        o2v = ht[:].rearrange("p (h d) -> p h d", h=8)
        beta, g_, G, GL, eG, kds, egs, bge, nbeta, ms = (sm[:, i, :] for i in range(10))

        def bch(a):
            return a.unsqueeze(2).to_broadcast([CH, 8, CH])
        for sc in range(n_sc):
            src = xh if sc * SC < TOK else x
            r0 = (sc * SC) % TOK
            x_ = xt[sc % 2]
            k.dma('sp' if sc % 2 == 0 else 'act', x_[:], src[r0:r0 + 128, :], writes=[x_])
            for hf in range(2):
                for kk in range(4):
                    kc = hf * 4 + kk
                    k.op('pe', lambda e: e.transpose(B[hf][:, kk * 128:(kk + 1) * 128], x_[:, kc * 128:(kc + 1) * 128], ident[:]),
                         reads=[x_, ident], writes=[B[hf]])
                k.op('act' if hf == 0 else 'dve',
                     lambda e: (e.copy if hf == 0 else e.tensor_copy)(xTs[:, hf * 4:hf * 4 + 4, :], B[hf][:].rearrange("p (k t) -> p k t", k=4)),
                     reads=[B[hf]], writes=[xTs])
            k.op('pool', lambda e: e.tensor_copy(tl[:], pre[:, :, SC:SC + 3]), reads=[pre], writes=[tl])
            for f4 in range(6):
                bk = B[2 + f4 % 2]
                for ff in range(4):
                    fb = f4 * 4 + ff
                    for kc in range(8):
                        k.mm(bk[:, ff * SC:(ff + 1) * SC], w_inb[:, kc, fb * 128:(fb + 1) * 128], xTs[:, kc, :], kc == 0, kc == 7,
                             reads=[w_inb, xTs], writes=[bk])
                k.op('act', lambda e: e.copy(pre[:, f4 * 4:f4 * 4 + 4, 3:SC + 3], bk[:].rearrange("p (f t) -> p f t", f=4)), reads=[bk, tl], writes=[pre])
            k.op('pool', lambda e: e.tensor_copy(pre[:, :, 0:3], tl[:]), reads=[tl], writes=[pre])
            for j in range(4):
                wj = cwT[:, j, :].unsqueeze(2).to_broadcast([128, 24, SC])
                if j == 0:
                    k.op('dve', lambda e: e.tensor_tensor(cv[:], pre[:, :, 0:SC], wj, ALU.mult), reads=[pre, cwT], writes=[cv])
                else:
                    k.op('pool', lambda e: e.tensor_tensor(cv2[:], pre[:, :, j:SC + j], wj, ALU.mult), reads=[pre, cwT], writes=[cv2])
                    k.op('dve', lambda e: e.tensor_tensor(cv[:], cv[:], cv2[:], ALU.add), reads=[cv, cv2], writes=[cv])
            k.op('act', lambda e: e.activation(cv[:], cv[:], AF.Silu), reads=[cv], writes=[cv])
            for cc in range(SC // CH):
                c_glob = sc * (SC // CH) + cc
                own = c_glob * CH >= TOK
                cols = slice(cc * CH, (cc + 1) * CH)
                for kc in range(8):
                    k.mm(B[4][0:CH, 0:16], xTs[:, kc, cols], w_inb[:, kc, 4096:4112], kc == 0, kc == 7, reads=[xTs, w_inb], writes=[B[4]])
                k.op('act', lambda e: e.activation(beta, B[4][0:CH, 0:8], AF.Sigmoid), reads=[B[4]], writes=[sm])
                k.op('dve', lambda e: e.tensor_tensor(g_, B[4][0:CH, 8:16], cst[:, 0:8], ALU.add), reads=[B[4], cst], writes=[sm])
                k.op('act', lambda e: e.activation(g_, g_, AF.Exp), reads=[sm], writes=[sm])
                k.op('dve', lambda e: e.tensor_scalar(g_, g_, 1.0, None, ALU.add), reads=[sm], writes=[sm])
                k.op('act', lambda e: e.activation(g_, g_, AF.Ln), reads=[sm], writes=[sm])
                k.op('dve', lambda e: e.tensor_tensor(g_, g_, cst[:, 8:16], ALU.mult), reads=[sm, cst], writes=[sm])
                k.mm(B[4][0:CH, 16:24], triU[:], g_, True, True, reads=[triU, sm], writes=[B[4]])
                k.mm(B[4][0:CH, 24:32], ones[0:CH, 0:CH], g_, True, True, reads=[ones, sm], writes=[B[4]])
                k.mm(B[4][:, 32:40], ones[0:CH, :], g_, True, True, reads=[ones, sm], writes=[B[4]])
                k.op('act', lambda e: e.copy(G, B[4][0:CH, 16:24]), reads=[B[4]], writes=[sm])
                k.op('act', lambda e: e.copy(GL, B[4][0:CH, 24:32]), reads=[B[4]], writes=[sm])
                k.op('act', lambda e: e.activation(sm128[:, 0:8], B[4][:, 32:40], AF.Exp), reads=[B[4]], writes=[sm128])
                k.op('act', lambda e: e.activation(eG, G, AF.Exp), reads=[sm], writes=[sm])
                k.op('dve', lambda e: e.tensor_tensor(kds, GL, G, ALU.subtract), reads=[sm], writes=[sm])
                k.op('act', lambda e: e.activation(kds, kds, AF.Exp), reads=[sm], writes=[sm])
                k.op('dve', lambda e: e.tensor_scalar(egs, eG, scale, None, ALU.mult), reads=[sm], writes=[sm])
                k.op('dve', lambda e: e.tensor_tensor(bge, beta, eG, ALU.mult), reads=[sm], writes=[sm])
                k.op('dve', lambda e: e.tensor_scalar(nbeta, beta, -1.0, None, ALU.mult), reads=[sm], writes=[sm])
                k.op('dve', lambda e: e.tensor_tensor(dg[:], ident[0:CH, 0:CH].unsqueeze(1).to_broadcast([CH, 8, CH]), bch(G), ALU.mult), reads=[ident, sm], writes=[dg])
                k.op('pool', lambda e: e.tensor_tensor(db[:], ident[0:CH, 0:CH].unsqueeze(1).to_broadcast([CH, 8, CH]), bch(beta), ALU.mult), reads=[ident, sm], writes=[db])
                k.mm(B[5][0:CH, :], ones[0:CH, 0:CH], dg[:].rearrange("p h f -> p (h f)"), True, True, reads=[ones, dg], writes=[B[5]])
                k.mm(B[6][0:CH, :], ones[0:CH, 0:CH], db[:].rearrange("p h f -> p (h f)"), True, True, reads=[ones, db], writes=[B[6]])
                Gb = B[5][0:CH, :].rearrange("p (h f) -> p h f", h=8)
                Bb = B[6][0:CH, :].rearrange("p (h f) -> p h f", h=8)
                k.op('dve', lambda e: e.scalar_tensor_tensor(E1[:], Gb, -1.0, bch(G), ALU.mult, ALU.add), reads=[B[5], sm], writes=[E1])
                k.op('dve', lambda e: e.tensor_scalar(E2[:], E1[:], -1.0, 0.0, ALU.mult, ALU.min), reads=[E1], writes=[E2])
                k.op('pool', lambda e: e.tensor_scalar(E1[:], E1[:], 0.0, None, ALU.min), reads=[E1], writes=[E1])
                k.op('act', lambda e: e.activation(E1[:], E1[:], AF.Exp), reads=[E1], writes=[E1])
                k.op('act', lambda e: e.activation(E2[:], E2[:], AF.Exp), reads=[E2], writes=[E2])
                k.op('dve', lambda e: e.scalar_tensor_tensor(E3[:], Bb, -1.0, E2[:], ALU.mult, ALU.mult), reads=[B[6], E2], writes=[E3])
                k.op('pool', lambda e: e.tensor_tensor(E3[:], E3[:], mUs[:], ALU.mult), reads=[E3, mUs], writes=[E3])
                k.op('dve', lambda e: e.tensor_tensor(E1[:], E1[:], bch(nbeta), ALU.mult), reads=[E1, sm], writes=[E1])
                k.op('pool', lambda e: e.tensor_tensor(E1[:], E1[:], mLs[:], ALU.mult), reads=[E1, mLs], writes=[E1])
                k.op('dve', lambda e: e.scalar_tensor_tensor(E2[:], E2[:], scale, mUi[:], ALU.mult, ALU.mult), reads=[E2, mUi], writes=[E2])
                k.op('dve', lambda e: e.tensor_tensor(sq[:], cv[:, 0:16, cols], cv[:, 0:16, cols], ALU.mult), reads=[cv], writes=[sq])
                for hf in range(2):
                    k.mm(B[hf][:], ones[:], sq[:, hf * 8:hf * 8 + 8, :].rearrange("p h t -> p (h t)"), True, True, reads=[ones, sq], writes=[B[hf]])
                    rv = rinv[:, hf * 8:hf * 8 + 8, :].rearrange("p h t -> p (h t)")
                    k.op('dve', lambda e: e.tensor_scalar(rv, B[hf][:], RMS_EPS, None, ALU.add), reads=[B[hf]], writes=[rinv])
                k.op('act', lambda e: e.activation(rinv[:], rinv[:], AF.Sqrt), reads=[rinv], writes=[rinv])
                k.op('dve', lambda e: e.reciprocal(rinv[:], rinv[:]), reads=[rinv], writes=[rinv])
                k.op('dve', lambda e: e.tensor_tensor(qkn[:], cv[:, 0:16, cols], rinv[:], ALU.mult), reads=[cv, rinv], writes=[qkn])
                for (dst, srcf) in ((ktm, lambda h: qkn[:, 8 + h, :]), (vtm, lambda h: cv[:, 16 + h, cols])):
                    for hf in range(2):
                        for hh in range(4):
                            h = hf * 4 + hh
                            k.op('pe', lambda e: e.transpose(B[hf][0:CH, hh * 128:(hh + 1) * 128], srcf(h), ident[:]), reads=[qkn, cv, ident], writes=[B[hf]])
                        k.op('act' if hf == 0 else 'dve',
                             lambda e: (e.copy if hf == 0 else e.tensor_copy)(dst[:, hf * 4:hf * 4 + 4, :], B[hf][0:CH, :].rearrange("p (h d) -> p h d", h=4)),
                             reads=[B[hf]], writes=[dst])
                for h in range(8):
                    k.mm(B[2][0:CH, h * CH:(h + 1) * CH], qkn[:, 8 + h, :], qkn[:, 8 + h, :], True, True, reads=[qkn], writes=[B[2]])
                    k.mm(B[3][0:CH, h * CH:(h + 1) * CH], qkn[:, 8 + h, :], qkn[:, h, :], True, True, reads=[qkn], writes=[B[3]])
                P1 = B[2][0:CH, :].rearrange("p (h f) -> p h f", h=8)
                k.op('dve', lambda e: e.tensor_tensor(A[:], P1, E1[:], ALU.mult), reads=[B[2], E1], writes=[A])
                k.op('dve', lambda e: e.tensor_tensor(AT[:], P1, E3[:], ALU.mult), reads=[B[2], E3], writes=[AT])
                k.op('dve', lambda e: e.tensor_tensor(IT[:], B[3][0:CH, :].rearrange("p (h f) -> p h f", h=8), E2[:], ALU.mult), reads=[B[3], E2], writes=[IT])
                k.op('dve', lambda e: e.tensor_tensor(X[:, :, 0:128], vtm[:], beta.unsqueeze(2).to_broadcast([CH, 8, 128]), ALU.mult), reads=[vtm, sm], writes=[X])
                k.op('pool', lambda e: e.tensor_tensor(X[:, :, 128:256], ktm[:], bge.unsqueeze(2).to_broadcast([CH, 8, 128]), ALU.mult), reads=[ktm, sm], writes=[X])
                k.op('pool', lambda e: e.tensor_tensor(kd[:], ktm[:], kds.unsqueeze(2).to_broadcast([CH, 8, 128]), ALU.mult), reads=[ktm, sm], writes=[kd])
                for lv in range(6):
                    for h in range(8):
                        k.mm(B[4 + h // 2][0:CH, (h % 2) * 256:(h % 2 + 1) * 256], AT[:, h, :], X[:, h, :], True, True, reads=[AT, X], writes=[B[4 + h // 2]])
                    if lv < 5:
                        for h in range(8):
                            k.mm(B[2][0:CH, h * CH:(h + 1) * CH], AT[:, h, :], A[:, h, :], True, True, reads=[AT, A], writes=[B[2]])
                            k.mm(B[3][0:CH, h * CH:(h + 1) * CH], A[:, h, :], AT[:, h, :], True, True, reads=[AT, A], writes=[B[3]])
                    for q4 in range(4):
                        xv = X[:, q4 * 2:q4 * 2 + 2, :].rearrange("p h d -> p (h d)")
                        k.op('dve', lambda e: e.tensor_tensor(xv, xv, B[4 + q4][0:CH, :], ALU.add), reads=[X, B[4 + q4]], writes=[X])
                    if lv < 5:
                        k.op('act', lambda e: e.copy(A[:].rearrange("p h f -> p (h f)"), B[2][0:CH, :]), reads=[B[2]], writes=[A])
                        k.op('act', lambda e: e.copy(AT[:].rearrange("p h f -> p (h f)"), B[3][0:CH, :]), reads=[B[3]], writes=[AT])
                for h in range(8):
                    k.op('pe', lambda e: e.transpose(B[0][:, h * CH:(h + 1) * CH], X[:, h, 128:256], ident[0:CH, 0:CH]), reads=[X, ident], writes=[B[0]])
                k.op('act', lambda e: e.copy(kcT[:].rearrange("p h c -> p (h c)"), B[0][:]), reads=[B[0]], writes=[kcT])
                if own:
                    for hf in range(2):
                        for kc in range(8):
                            k.mm(B[6 + hf][0:CH, :], xTs[:, kc, cols], w_inb[:, kc, 3072 + hf * 512:3072 + (hf + 1) * 512], kc == 0, kc == 7,
                                 reads=[xTs, w_inb], writes=[B[6 + hf]])
                        k.op('act', lambda e: e.activation(zs[:, hf * 4:hf * 4 + 4, :].rearrange("p h d -> p (h d)"), B[6 + hf][0:CH, :], AF.Silu),
                             reads=[B[6 + hf]], writes=[zs])
                for h0 in range(0, 8, 2):
                    hs2 = (h0, h0 + 1)
                    bk = {h0: (B[1], B[2], B[3], B[0]), h0 + 1: (B[5], B[6], B[7], B[4])}
                    for h in hs2:
                        k.mm(bk[h][0][0:CH, 0:128], kcT[:, h, :], S[:, h, :], True, True, reads=[kcT, S_t[h]], writes=[bk[h][0]])
                    for h in hs2:
                        k.op('dve', lambda e: e.tensor_tensor(vn[h % 2][:], X[:, h, 0:128], bk[h][0][0:CH, 0:128], ALU.subtract),
                             reads=[X, bk[h][0]], writes=[vn[h % 2]])
                    if own:
                        for h in hs2:
                            k.mm(bk[h][1][0:CH, 0:128], qkn[:, h, :], S[:, h, :], True, True, reads=[qkn, S_t[h]], writes=[bk[h][1]])
                            k.mm(bk[h][2][0:CH, 0:128], IT[:, h, :], vn[h % 2][:], True, True, reads=[IT, vn[h % 2]], writes=[bk[h][2]])
                        for h in hs2:
                            k.op('act', lambda e: e.activation(qs[h % 2][:], bk[h][1][0:CH, 0:128], AF.Copy, scale=egs[:, h:h + 1]),
                                 reads=[bk[h][1], sm], writes=[qs[h % 2]])
                        for h in hs2:
                            k.op('dve', lambda e: e.tensor_tensor(o[:, h, :], qs[h % 2][:], bk[h][2][0:CH, 0:128], ALU.add),
                                 reads=[qs[h % 2], bk[h][2]], writes=[o])
                    for h in hs2:
                        k.mm(bk[h][3][:, 0:128], kd[:, h, :], vn[h % 2][:], True, True, reads=[kd, vn[h % 2]], writes=[bk[h][3]])
                    for h in hs2:
                        k.op('dve', lambda e: e.scalar_tensor_tensor(S[:, h, :], S[:, h, :], sm128[:, h:h + 1], bk[h][3][:, 0:128], ALU.mult, ALU.add),
                             reads=[S_t[h], sm128, bk[h][3]], writes=[S_t[h]])
                if own:
                    k.op('pool', lambda e: e.tensor_tensor(o2v, o[:], o[:], ALU.mult), reads=[o], writes=[ht])
                    k.op('dve', lambda e: e.tensor_reduce(ms, o2v, AX.X, ALU.add), reads=[ht], writes=[sm])
                    k.op('dve', lambda e: e.tensor_scalar(ms, ms, 1.0 / 128, RMS_EPS, ALU.mult, ALU.add), reads=[sm], writes=[sm])
                    k.op('act', lambda e: e.activation(ms, ms, AF.Sqrt), reads=[sm], writes=[sm])
                    k.op('dve', lambda e: e.reciprocal(ms, ms), reads=[sm], writes=[sm])
                    k.op('dve', lambda e: e.tensor_tensor(o[:], o[:], ms.unsqueeze(2).to_broadcast([CH, 8, 128]), ALU.mult), reads=[o, sm], writes=[o])
                    k.op('pool', lambda e: e.tensor_tensor(o[:], o[:], nwb[:].unsqueeze(1).to_broadcast([CH, 8, 128]), ALU.mult), reads=[o, nwb], writes=[o])
                    k.op('dve', lambda e: e.tensor_tensor(o[:], o[:], zs[:], ALU.mult), reads=[o, zs], writes=[o])
                    for h in range(8):
                        k.op('pe', lambda e: e.transpose(B[4][:, h * CH:(h + 1) * CH], o[:, h, :], ident[0:CH, 0:CH]), reads=[o, ident], writes=[B[4]])
                    k.op('act', lambda e: e.copy(ogT[:].rearrange("p h c -> p (h c)"), B[4][:]), reads=[B[4]], writes=[ogT])
                    for dh in range(2):
                        for kc in range(8):
                            k.mm(B[5 + dh][0:CH, :], ogT[:, kc, :], w_outb[:, kc, dh * 512:(dh + 1) * 512], kc == 0, kc == 7,
                                 reads=[ogT, w_outb], writes=[B[5 + dh]])
                        k.op('act', lambda e: e.copy(ht[:, dh * 512:(dh + 1) * 512], B[5 + dh][0:CH, :]), reads=[B[5 + dh]], writes=[ht])
                    t0 = c_glob * CH - TOK
                    k.dma('sp', hs[t0:t0 + CH, :], ht[:], reads=[ht], writes=[hs])
    post(k, x, hs, io['lng'], io['lnb'], io['wr'], io['idn'], io['x1o'], io['x1T'], io['gto'])
    return tail(k, io)


_BUILD_CACHE = {}


def _get(name, fn):
    if name not in _BUILD_CACHE:
        _BUILD_CACHE[name] = fn()
    return _BUILD_CACHE[name]


def _run(nc, in_maps):
    res = run_bass_kernel_spmd(nc, in_maps, core_ids=list(range(NCORES)))
    return res.results


def kernel(x, rel_bias, a_w_in, a_w_out, b_w_in, b_norm_g, b_norm_b, b_w_s, b_b_s, b_w_out,
           c_w_in, c_conv, c_a_log, c_dt_bias, c_norm_w, c_w_out, ln_g, ln_b,
           moe_w_coarse, moe_w_fine, moe_w_gate, moe_w_up, moe_w_down):
    f32 = lambda a: np.ascontiguousarray(np.asarray(a), dtype=np.float32)
    x = f32(x)
    Bn, S, Dm = x.shape
    xs = x.reshape(Bn * S, Dm)
    cur = [xs[c * TOK:(c + 1) * TOK] for c in range(NCORES)]
    idn = np.eye(128, dtype=np.float32)
    zeros = np.zeros((TOK, D), np.float32)
    tabs = att_tables(f32(rel_bias))
    negf = [np.full((128, 128), NEG, np.float32), np.zeros((128, 128), np.float32)]
    depth = ln_g.shape[0]
    for i in range(depth):
        kind, j = i % 3, i // 3
        wr = np.ascontiguousarray(np.concatenate([f32(moe_w_coarse[i]), f32(moe_w_fine[i]).reshape(D, 64)], axis=1))
        pio = dict(lng=f32(ln_g[i, 0]), lnb=f32(ln_b[i, 0]), wr=wr, idn=idn,
                   wg=f32(moe_w_gate[i]), wu=f32(moe_w_up[i]), wd=f32(moe_w_down[i]),
                   lng2=f32(ln_g[i, 1]), lnb2=f32(ln_b[i, 1]))
        halo = [cur[c - 1] if c % 2 == 1 else zeros for c in range(NCORES)]
        if kind == 0:
            nc = build_M_att()
            w_in, w_out = f32(a_w_in[j]), f32(a_w_out[j])
            maps = [dict(x=cur[c], xh=halo[c], w_in=w_in, w_out=w_out, tab=tabs, negf=negf[c % 2], **pio) for c in range(NCORES)]
        elif kind == 1:
            nc = build_M_sgu()
            ws = dict(w_in=f32(b_w_in[j]), ng=f32(b_norm_g[j]), nb=f32(b_norm_b[j]), w_s=f32(b_w_s[j]),
                      b_s=f32(b_b_s[j]).reshape(2048), w_out=f32(b_w_out[j]))
            maps = [dict(x=cur[c], **ws, **pio) for c in range(NCORES)]
        else:
            nc = build_M_gdn()
            ws = dict(w_in=f32(c_w_in[j]), conv=f32(c_conv[j]), a_log=f32(c_a_log[j]), dt_b=f32(c_dt_bias[j]),
                      norm_w=f32(c_norm_w[j]), w_out=f32(c_w_out[j]))
            maps = [dict(x=cur[c], xh=halo[c], **ws, **pio) for c in range(NCORES)]
        res = _run(nc, maps)
        cur = [np.asarray(r['x2']) for r in res]
    return np.concatenate(cur, axis=0).reshape(Bn, S, Dm).astype(np.float32)
```
